# Optimizing a Trainium2 kernel written in Bass

```python
import math
import jax, jax.numpy as jnp
from jax import lax
import numpy as np

D_MODEL = 2048
BATCH = 2
SEQ = 8192
DEPTH = 1

CHUNK = 64
Q_BLOCK = 128
HEAD_DIM = 128
N_SB_HEADS = 8
N_DIFF_HEADS = 4
SB_WIDTH = N_SB_HEADS * HEAD_DIM
DIFF_WIDTH = N_DIFF_HEADS * 2 * HEAD_DIM
D_MIX = SB_WIDTH + DIFF_WIDTH
IN_COLS = 3 * SB_WIDTH + 3 * DIFF_WIDTH
ROPE_THETA = 10000.0
N_GROUPS = 4
EXPERTS_PER_GROUP = 8
N_EXPERTS = N_GROUPS * EXPERTS_PER_GROUP
D_EXPERT = 512
TOP_K_INNER = 2
NORM_EPS = 1e-6
NEG_INF = -1e30

kernel_name = "hybrid_stickbreak_diffattn_hmoe"


def rms_norm(x, gain):
    xf = x.astype(jnp.float32)
    y = xf * lax.rsqrt(jnp.mean(xf * xf, axis=-1, keepdims=True) + NORM_EPS)
    return (y * gain.astype(jnp.float32)).astype(x.dtype)


def lambda_init(layer_idx):
    return 0.8 - 0.6 * math.exp(-0.3 * layer_idx)


def rotary(x, pos):
    half = x.shape[-1] // 2
    inv_freq = 1.0 / (ROPE_THETA ** (jnp.arange(half, dtype=jnp.float32) / half))
    ang = pos.astype(jnp.float32)[:, None] * inv_freq[None, :]
    cos = jnp.cos(ang)[None, :, None, :]
    sin = jnp.sin(ang)[None, :, None, :]
    xf = x.astype(jnp.float32)
    x1, x2 = xf[..., :half], xf[..., half:]
    out = jnp.concatenate([x1 * cos - x2 * sin, x2 * cos + x1 * sin], axis=-1)
    return out.astype(x.dtype)


def sweep_query_blocks(block_fn, seq):
    n_blocks = seq // Q_BLOCK
    out = lax.map(block_fn, jnp.arange(n_blocks))
    nb, b, qb, h, e = out.shape
    return out.transpose(1, 0, 2, 3, 4).reshape(b, nb * qb, h, e)


def stick_breaking_attention(q, k, v):
    seq, d = q.shape[1], q.shape[-1]
    scale = 1.0 / math.sqrt(d)
    kpos = jnp.arange(seq)

    def block(i):
        start = i * Q_BLOCK
        qb = lax.dynamic_slice_in_dim(q, start, Q_BLOCK, axis=1)
        z = jnp.einsum('bqhd,bkhd->bhqk', qb, k).astype(jnp.float32) * scale
        qpos = start + jnp.arange(Q_BLOCK)
        strict = kpos[None, :] < qpos[:, None]
        log_beta = jax.nn.log_sigmoid(z)
        log_keep = jnp.where(strict, log_beta - z, 0.0)
        later = lax.cumsum(log_keep, axis=3, reverse=True) - log_keep
        a = jnp.where(strict, jnp.exp(log_beta + later), 0.0)
        return jnp.einsum('bhqk,bkhd->bqhd', a.astype(v.dtype), v)

    return sweep_query_blocks(block, seq)


def differential_attention(q1, q2, k1, k2, v, lam):
    seq, d = q1.shape[1], q1.shape[-1]
    scale = 1.0 / math.sqrt(d)
    kchunk = jnp.arange(seq) // CHUNK

    def block(i):
        start = i * Q_BLOCK
        qb1 = lax.dynamic_slice_in_dim(q1, start, Q_BLOCK, axis=1)
        qb2 = lax.dynamic_slice_in_dim(q2, start, Q_BLOCK, axis=1)
        qchunk = (start + jnp.arange(Q_BLOCK)) // CHUNK
        mask = kchunk[None, :] <= qchunk[:, None]
        s1 = jnp.einsum('bqhd,bkhd->bhqk', qb1, k1).astype(jnp.float32) * scale
        s2 = jnp.einsum('bqhd,bkhd->bhqk', qb2, k2).astype(jnp.float32) * scale
        p1 = jax.nn.softmax(jnp.where(mask, s1, NEG_INF), axis=-1)
        p2 = jax.nn.softmax(jnp.where(mask, s2, NEG_INF), axis=-1)
        a = p1 - lam * p2
        return jnp.einsum('bhqk,bkhe->bqhe', a.astype(v.dtype), v)

    return sweep_query_blocks(block, seq)


def hierarchical_moe(h, w_group_router, b_group_router, w_expert_router,
                     b_expert_router, w_gate, w_up, w_down):
    b, s, d = h.shape
    t = h.reshape(b * s, d)
    g_prob = jax.nn.softmax((t @ w_group_router).astype(jnp.float32)
                            + b_group_router.astype(jnp.float32), axis=-1)
    g_val, g_idx = lax.top_k(g_prob, 1)
    e_all = (t @ w_expert_router).astype(jnp.float32) + b_expert_router.astype(jnp.float32)
    e_all = e_all.reshape(-1, N_GROUPS, EXPERTS_PER_GROUP)
    g_onehot = jax.nn.one_hot(g_idx[:, 0], N_GROUPS, dtype=jnp.float32)
    e_logits = jnp.sum(e_all * g_onehot[:, :, None], axis=1)
    e_val, e_idx = lax.top_k(jax.nn.softmax(e_logits, axis=-1), TOP_K_INNER)
    e_val = e_val / jnp.sum(e_val, axis=-1, keepdims=True)
    weights = g_val * e_val
    global_idx = g_idx * EXPERTS_PER_GROUP + e_idx
    combine = jnp.sum(jax.nn.one_hot(global_idx, N_EXPERTS, dtype=jnp.float32)
                      * weights[..., None], axis=1)

    def expert_step(acc, xs):
        wg, wu, wd, c = xs
        hid = jax.nn.silu(t @ wg) * (t @ wu)
        return acc + c[:, None] * (hid @ wd).astype(jnp.float32), None

    acc, _ = lax.scan(expert_step, jnp.zeros((b * s, d), jnp.float32),
                      (w_gate, w_up, w_down, combine.T))
    return acc.astype(h.dtype).reshape(b, s, d)


def setup_inputs(seed: int = 0) -> dict:
    key = jax.random.key(seed)
    ks = jax.random.split(key, 20)
    f32 = jnp.float32
    nrm = lambda k, shape, sc: (jax.random.normal(k, shape, f32) * sc)
    return {
        "x": nrm(ks[0], (BATCH, SEQ, D_MODEL), 1.0),
        "attn_norm_gain": 1.0 + nrm(ks[1], (DEPTH, D_MODEL), 0.02),
        "w_in": nrm(ks[2], (DEPTH, D_MODEL, IN_COLS), D_MODEL ** -0.5),
        "sb_norm_gain": 1.0 + nrm(ks[3], (DEPTH, HEAD_DIM), 0.02),
        "diff_lambda_q1": nrm(ks[4], (DEPTH, HEAD_DIM), 0.1),
        "diff_lambda_k1": nrm(ks[5], (DEPTH, HEAD_DIM), 0.1),
        "diff_lambda_q2": nrm(ks[6], (DEPTH, HEAD_DIM), 0.1),
        "diff_lambda_k2": nrm(ks[7], (DEPTH, HEAD_DIM), 0.1),
        "diff_subln_gain": 1.0 + nrm(ks[8], (DEPTH, 2 * HEAD_DIM), 0.02),
        "w_out": nrm(ks[9], (DEPTH, D_MIX, D_MODEL), D_MIX ** -0.5),
        "ffn_norm_gain": 1.0 + nrm(ks[10], (DEPTH, D_MODEL), 0.02),
        "w_group_router": nrm(ks[11], (DEPTH, D_MODEL, N_GROUPS), D_MODEL ** -0.5),
        "b_group_router": nrm(ks[12], (DEPTH, N_GROUPS), 0.01),
        "w_expert_router": nrm(ks[13], (DEPTH, D_MODEL, N_EXPERTS), D_MODEL ** -0.5),
        "b_expert_router": nrm(ks[14], (DEPTH, N_EXPERTS), 0.01),
        "w_gate": nrm(ks[15], (DEPTH, N_EXPERTS, D_MODEL, D_EXPERT), D_MODEL ** -0.5),
        "w_up": nrm(ks[16], (DEPTH, N_EXPERTS, D_MODEL, D_EXPERT), D_MODEL ** -0.5),
        "w_down": nrm(ks[17], (DEPTH, N_EXPERTS, D_EXPERT, D_MODEL), D_EXPERT ** -0.5),
        "final_norm_gain": 1.0 + nrm(ks[18], (D_MODEL,), 0.02),
    }


def reference(x, attn_norm_gain, w_in, sb_norm_gain, diff_lambda_q1, diff_lambda_k1,
              diff_lambda_q2, diff_lambda_k2, diff_subln_gain, w_out, ffn_norm_gain,
              w_group_router, b_group_router, w_expert_router, b_expert_router,
              w_gate, w_up, w_down, final_norm_gain):
    b, s, _ = x.shape
    pos = jnp.arange(s)
    for layer in range(DEPTH):
        h = rms_norm(x, attn_norm_gain[layer])
        proj = h @ w_in[layer]
        o = 0
        sb_q = proj[..., o:o + SB_WIDTH].reshape(b, s, N_SB_HEADS, HEAD_DIM); o += SB_WIDTH
        sb_k = proj[..., o:o + SB_WIDTH].reshape(b, s, N_SB_HEADS, HEAD_DIM); o += SB_WIDTH
        sb_v = proj[..., o:o + SB_WIDTH].reshape(b, s, N_SB_HEADS, HEAD_DIM); o += SB_WIDTH
        dq = proj[..., o:o + DIFF_WIDTH].reshape(b, s, N_DIFF_HEADS, 2, HEAD_DIM); o += DIFF_WIDTH
        dk = proj[..., o:o + DIFF_WIDTH].reshape(b, s, N_DIFF_HEADS, 2, HEAD_DIM); o += DIFF_WIDTH
        dv = proj[..., o:o + DIFF_WIDTH].reshape(b, s, N_DIFF_HEADS, 2 * HEAD_DIM)

        sb_out = stick_breaking_attention(sb_q, sb_k, sb_v)
        sb_out = rms_norm(sb_out, sb_norm_gain[layer]).reshape(b, s, SB_WIDTH)

        lam_init = lambda_init(layer)
        lam = (jnp.exp(jnp.sum(diff_lambda_q1[layer].astype(jnp.float32) * diff_lambda_k1[layer].astype(jnp.float32)))
               - jnp.exp(jnp.sum(diff_lambda_q2[layer].astype(jnp.float32) * diff_lambda_k2[layer].astype(jnp.float32)))
               + lam_init)
        q1 = rotary(dq[..., 0, :], pos)
        q2 = rotary(dq[..., 1, :], pos)
        k1 = rotary(dk[..., 0, :], pos)
        k2 = rotary(dk[..., 1, :], pos)
        d_out = differential_attention(q1, q2, k1, k2, dv, lam)
        d_out = (rms_norm(d_out, diff_subln_gain[layer]) * (1.0 - lam_init)).astype(x.dtype)
        d_out = d_out.reshape(b, s, DIFF_WIDTH)

        mixed = jnp.concatenate([sb_out, d_out], axis=-1)
        x = x + mixed @ w_out[layer]

        h = rms_norm(x, ffn_norm_gain[layer])
        x = x + hierarchical_moe(h, w_group_router[layer], b_group_router[layer],
                                 w_expert_router[layer], b_expert_router[layer],
                                 w_gate[layer], w_up[layer], w_down[layer])
    return rms_norm(x, final_norm_gain)
```

```python
import os
import math
import numpy as np
from contextlib import ExitStack
import concourse.bass as bass
import concourse.mybir as mybir
from concourse.bass_utils import run_bass_kernel_spmd

F32 = mybir.dt.float32
BF16 = mybir.dt.bfloat16
I32 = mybir.dt.int32
AF = mybir.ActivationFunctionType
ALU = mybir.AluOpType
AX = mybir.AxisListType

SEQ = 8192
DM = 2048
NTOK = 2048
NE = 32
DE = 512
EPS = 1e-6
SCALE = 1.0 / math.sqrt(128.0)
LAM_INIT = 0.8 - 0.6 * math.exp(0.0)
PI = math.pi

STAGE = int(os.environ.get("MK_STAGE", "99"))
C1T = int(os.environ.get("MK_C1T", "16"))
C1R = int(os.environ.get("MK_C1R", "2"))


class _One(list):
    def __getitem__(self, i):
        return list.__getitem__(self, 0)

    def __setitem__(self, i, v):
        list.__setitem__(self, 0, v)


class DS:
    def __init__(self, sem):
        self.sem = sem
        self.count = 0


class Sched:
    def __init__(self, nc, es):
        self.nc = nc
        self.es = es
        self.eng = {'sync': nc.sync, 'act': nc.scalar, 'pe': nc.tensor, 'dve': nc.vector, 'pool': nc.gpsimd}
        self.sem = {k: es.enter_context(nc.semaphore("cs_" + k)) for k in ['act', 'pe', 'dve', 'pool']}
        self.cnt = {k: 0 for k in self.sem}
        self.seen = {}
        self.nds = 0
        self.pending = []

    def dsem(self):
        self.nds += 1
        return DS(self.es.enter_context(self.nc.semaphore("ds%d" % self.nds)))

    def _wait(self, eng, deps):
        need = {}
        for d in deps:
            if d is None:
                continue
            sem, val = d
            if val > need.get(sem, (None, 0))[1]:
                need[sem] = (sem, val)
        for sem, val in need.values():
            if self.seen.get((eng, sem), 0) >= val:
                continue
            self.eng[eng].wait_ge(sem, val)
            self.seen[(eng, sem)] = val

    def op(self, eng, fn, deps=(), signal=True, seq=False):
        deps = list(deps)
        if seq and self.cnt[eng] > 0:
            deps.append((self.sem[eng], self.cnt[eng]))
        self._wait(eng, deps)
        ins = fn(self.eng[eng])
        if signal or eng != 'pe':
            self.cnt[eng] += 1
            ins.then_inc(self.sem[eng], 1)
            return (self.sem[eng], self.cnt[eng])
        return None

    def dma(self, q, out, in_, ds, deps=()):
        self._wait(q, deps)
        ds.count += 16
        self.eng[q].dma_start(out=out, in_=in_).then_inc(ds.sem, 16)
        tok = (ds.sem, ds.count)
        return tok

    def last(self, eng):
        return (self.sem[eng], self.cnt[eng])

    def fence(self, extra=()):
        toks = [self.last(k) for k in self.sem] + list(extra)
        for e in ['sync', 'act', 'pe', 'dve', 'pool']:
            self._wait(e, toks)


def rstd_chain(s, dst, src, inv_n, deps):
    t1 = s.op('dve', lambda e: e.tensor_scalar(out=dst, in0=src, scalar1=inv_n, scalar2=EPS, op0=ALU.mult, op1=ALU.add), deps)
    t2 = s.op('act', lambda e: e.activation(out=dst, in_=dst, func=AF.Ln), [t1])
    t3 = s.op('act', lambda e: e.activation(out=dst, in_=dst, func=AF.Exp, scale=-0.5), [t2])
    return t3


def build_program():
    nc = bass.Bass("TRN2", target_bir_lowering=False)
    dt = lambda name, shape, dtype, kind: nc.dram_tensor(name, shape, dtype, kind=kind).ap()
    xb = dt("xb", [SEQ, DM], F32, "ExternalInput")
    xs = dt("xs", [NTOK, DM], F32, "ExternalInput")
    win = dt("win", [DM, 2048], F32, "ExternalInput")
    g1 = dt("g1", [1, DM], F32, "ExternalInput")
    g2 = dt("g2", [1, DM], F32, "ExternalInput")
    g3 = dt("g3", [1, DM], F32, "ExternalInput")
    sbg = dt("sbg", [128, 1], F32, "ExternalInput")
    dsg = dt("dsg", [128, 2], F32, "ExternalInput")
    lam4 = dt("lam4", [4, 128], F32, "ExternalInput")
    wout = dt("wout", [DM, DM], F32, "ExternalInput")
    wr = dt("wr", [DM, 36], F32, "ExternalInput")
    br = dt("br", [1, 36], F32, "ExternalInput")
    wgm = dt("wgm", [8 * DM, DE], F32, "ExternalInput")
    wum = dt("wum", [8 * DM, DE], F32, "ExternalInput")
    wdm = dt("wdm", [8 * DE, DM], F32, "ExternalInput")
    sel = dt("sel", [1, 4], F32, "ExternalInput")
    y = dt("y", [NTOK, DM], F32, "ExternalOutput")
    cosT = dt("cosT", [128, SEQ], F32, "Internal")
    sinT = dt("sinT", [128, SEQ], F32, "Internal")
    DBG = "ExternalOutput" if STAGE <= 3 else "Internal"
    DBG2 = "ExternalOutput" if STAGE <= 4 else "Internal"
    QK = dt("QK", [8, 128, SEQ], BF16, DBG)
    VV = dt("VV", [SEQ, 512], BF16, DBG)
    mix_loc = dt("mix_loc", [16, 512, 512], BF16, DBG)
    mix_all = dt("mix_all", [16, 2048, 512], BF16, "Internal")
    X1 = dt("X1", [NTOK, DM], F32, DBG2)
    H2T = dt("H2T", [DM, NTOK], BF16, DBG2)
    Wgu_loc = dt("Wgu_loc", [32, 1024, DE], BF16, "Internal")
    Wgu_all = dt("Wgu_all", [32, 4096, DE], BF16, "Internal")
    Wd_loc = dt("Wd_loc", [16, 256, DM], BF16, "Internal")
    Wd_all = dt("Wd_all", [16, 1024, DM], BF16, "Internal")

    with ExitStack() as es:
        s = Sched(nc, es)
        sbp = lambda name, shape, dtype: es.enter_context(nc.sbuf_tensor(name, shape, dtype))
        ps_all = es.enter_context(nc.psum_tensor("ps_all", [128, 8 * 512], F32))
        bank = lambda i: ps_all[:, i * 512:(i + 1) * 512]
        cc_sem = es.enter_context(nc.semaphore("cc_sem"))
        cc_cnt = [0]

        pidx = sbp("pidx", [128, 1], F32)
        identf = sbp("identf", [128, 128], F32)
        identb = sbp("identb", [128, 128], BF16)
        negU = sbp("negU", [128, 128], BF16)
        negOnes = sbp("negOnes", [128, 128], BF16)
        onesf = sbp("onesf", [128, 128], F32)
        msb = sbp("msb", [128, 4, 512], BF16)
        mdf = sbp("mdf", [128, 4, 512], BF16)
        thr = sbp("thr", [128, 5], F32)
        sbg_t = sbp("sbg_t", [128, 1], F32)
        dsg_t = sbp("dsg_t", [128, 2], F32)
        gsc = sbp("gsc", [128, 2], F32)
        lamb = sbp("lamb", [128, 4, 128], F32)
        lamw = sbp("lamw", [128, 2, 128], F32)
        lams = sbp("lams", [128, 4], F32)
        neglam = sbp("neglam", [128, 1], F32)
        es_c = ExitStack()
        tmpc = lambda name, shape, dtype: es_c.enter_context(nc.sbuf_tensor(name, shape, dtype))
        it_i = tmpc("it_i", [128, 512], I32)
        it_f = tmpc("it_f", [128, 512], F32)
        tv_i = tmpc("tv_i", [128, 512], I32)
        tv_f = tmpc("tv_f", [128, 512], F32)
        pid_i = tmpc("pid_i", [128, 1], I32)

        t0 = s.op('pool', lambda e: e.iota(it_i[:], pattern=[[1, 512]], base=0, channel_multiplier=-1))
        t1 = s.op('pool', lambda e: e.iota(tv_i[:], pattern=[[1, 512]], base=0, channel_multiplier=0))
        t2 = s.op('pool', lambda e: e.iota(pid_i[:], pattern=[[0, 1]], base=0, channel_multiplier=1))
        c0 = s.op('dve', lambda e: e.tensor_copy(out=it_f[:], in_=it_i[:]), [t0])
        c1 = s.op('dve', lambda e: e.tensor_copy(out=tv_f[:], in_=tv_i[:]), [t1])
        c2 = s.op('dve', lambda e: e.tensor_copy(out=pidx[:], in_=pid_i[:]), [t2])
        s.op('dve', lambda e: e.tensor_single_scalar(out=identf[:], in_=it_f[:, 0:128], scalar=0.0, op=ALU.is_equal), [c0])
        s.op('dve', lambda e: e.tensor_single_scalar(out=identb[:], in_=it_f[:, 0:128], scalar=0.0, op=ALU.is_equal), [c0])
        s.op('dve', lambda e: e.tensor_scalar(out=negU[:], in0=it_f[:, 0:128], scalar1=0.0, scalar2=-1.0,
                                              op0=ALU.is_le, op1=ALU.mult), [c0])
        s.op('dve', lambda e: e.memset(negOnes[:], -1.0))
        s.op('dve', lambda e: e.memset(onesf[:], 1.0))
        for j in range(4):
            s.op('dve', lambda e, j=j: e.tensor_single_scalar(out=msb[:, j, :], in_=it_f[:], scalar=128.0 * j,
                                                              op=ALU.is_gt), [c0])
        s.op('dve', lambda e: e.tensor_scalar(out=thr[:, 4:5], in0=pidx[:], scalar1=64.0, scalar2=64.0,
                                              op0=ALU.is_ge, op1=ALU.mult), [c2])
        for j in range(4):
            tj = s.op('dve', lambda e, j=j: e.tensor_scalar_add(out=thr[:, j:j + 1], in0=thr[:, 4:5], scalar1=128.0 * j),
                      [s.last('dve')])
            s.op('dve', lambda e, j=j: e.tensor_scalar(out=mdf[:, j, :], in0=tv_f[:], scalar1=thr[:, j:j + 1], scalar2=None,
                                                       op0=ALU.is_ge), [tj, c1])
        cst_tok = s.last('dve')
        es_c.close()
        s.fence()

        dsP = s.dsem()
        s.dma('sync', sbg_t[:], sbg, dsP)
        s.dma('sync', dsg_t[:], dsg, dsP)
        for i in range(4):
            s.dma('sync', lamb[:, i, :], lam4[i:i + 1, :].partition_broadcast(128), dsP)
        ptok = (dsP.sem, dsP.count)
        s.op('dve', lambda e: e.tensor_scalar_mul(out=gsc[:], in0=dsg_t[:], scalar1=1.0 - LAM_INIT), [ptok])
        s.op('dve', lambda e: e.tensor_tensor(out=lamw[:, 0, :], in0=lamb[:, 0, :], in1=lamb[:, 1, :], op=ALU.mult), [ptok])
        s.op('dve', lambda e: e.tensor_tensor(out=lamw[:, 1, :], in0=lamb[:, 2, :], in1=lamb[:, 3, :], op=ALU.mult), [ptok])
        l0 = s.op('dve', lambda e: e.reduce_sum(out=lams[:, 0:2], in_=lamw[:], axis=AX.X), [s.last('dve')])
        l1 = s.op('act', lambda e: e.activation(out=lams[:, 2:4], in_=lams[:, 0:2], func=AF.Exp), [l0])
        l2 = s.op('dve', lambda e: e.tensor_tensor(out=neglam[:], in0=lams[:, 3:4], in1=lams[:, 2:3], op=ALU.subtract), [l1])
        s.op('dve', lambda e: e.tensor_scalar_add(out=neglam[:], in0=neglam[:], scalar1=-LAM_INIT), [l2])

        es_cv = ExitStack()
        sb2c = lambda name, shape, dtype: es_cv.enter_context(nc.sbuf_tensor(name, shape, dtype))
        stg = [sb2c("cv_f%d" % i, [128, 4, 512], F32) for i in range(2)]
        obf = [sb2c("cv_b%d" % i, [128, 4, 512], BF16) for i in range(2)]
        dsl_c = [s.dsem() for _ in range(3)]
        dso_c = [s.dsem() for _ in range(3)]
        cast_eng = ['pool', 'act']
        jobs = []
        Wgu_flat = Wgu_loc.rearrange("a r c -> (a r) c")
        Wd_flat = Wd_loc.rearrange("a r c -> (a r) c")
        for i in range(8):
            for m, srct in enumerate([wgm, wum]):
                for q in range(4):
                    ch = i * 4 + q
                    srcv = srct[ch * 512:(ch + 1) * 512, :].rearrange("(r p) c -> p r c", p=128)
                    r0 = (i * 2 + m) * DM + q * 512
                    dstv = Wgu_flat[r0:r0 + 512, :].rearrange("(r p) c -> p r c", p=128)
                    jobs.append((srcv, dstv))
            for q in range(4):
                ch = i * 4 + q
                srcv = wdm[ch * 128:(ch + 1) * 128, :].rearrange("p (r c) -> p r c", c=512)
                dstv = Wd_flat[ch * 128:(ch + 1) * 128, :].rearrange("p (r c) -> p r c", c=512)
                jobs.append((srcv, dstv))
        cast_tok = [None] * 2
        out_tok = [None] * 2
        jobctr = [0]
        GRP4 = [[0, 1, 2, 3], [4, 5, 6, 7]]

        def conv_step(nj):
            for _ in range(nj):
                n = jobctr[0]
                if n >= len(jobs):
                    return
                jobctr[0] += 1
                srcv, dstv = jobs[n]
                k = n % 2
                ld = s.dma('sync', stg[k][:], srcv, dsl_c[k], [cast_tok[k]])
                ct = s.op('pool', lambda e, k=k: e.tensor_copy(out=obf[k][:], in_=stg[k][:]), [ld, out_tok[k]])
                cast_tok[k] = ct
                out_tok[k] = s.dma('sync', dstv, obf[k][:], dso_c[k], [ct])
                if n % 12 == 11:
                    i = n // 12
                    s._wait('pool', [t for t in out_tok if t is not None])
                    for q in range(4):
                        nc.gpsimd.collective_compute("AllGather", ALU.bypass, replica_groups=GRP4,
                                                     ins=[Wgu_loc[i * 4 + q]], outs=[Wgu_all[i * 4 + q]]).then_inc(cc_sem)
                        cc_cnt[0] += 1
                    for q in range(2):
                        nc.gpsimd.collective_compute("AllGather", ALU.bypass, replica_groups=GRP4,
                                                     ins=[Wd_loc[i * 2 + q]], outs=[Wd_all[i * 2 + q]]).then_inc(cc_sem)
                        cc_cnt[0] += 1

        def conv_finish():
            conv_step(len(jobs))
            return (cc_sem, cc_cnt[0])

        with ExitStack() as es2:
            sb2 = lambda name, shape, dtype: es2.enter_context(nc.sbuf_tensor(name, shape, dtype))
            pm = sb2("pm", [128, 1], F32)
            invf = sb2("invf", [128, 1], F32)
            sgn = sb2("sgn", [128, 1], F32)
            posi = sb2("posi", [128, 2048], I32)
            posf = sb2("posf", [128, 2048], F32)
            ang = sb2("ang", [128, 2048], F32)
            r1 = sb2("r1", [128, 2048], F32)
            r2 = sb2("r2", [128, 2048], F32)
            sn = sb2("sn", [128, 2048], F32)
            cs = sb2("cs", [128, 2048], F32)
            dsr = s.dsem()
            s.op('dve', lambda e: e.tensor_scalar(out=pm[:], in0=pidx[:], scalar1=64.0, scalar2=-64.0, op0=ALU.is_ge, op1=ALU.mult), [c2])
            a0 = s.op('dve', lambda e: e.tensor_tensor(out=pm[:], in0=pm[:], in1=pidx[:], op=ALU.add), [s.last('dve')])
            a1 = s.op('act', lambda e: e.activation(out=invf[:], in_=pm[:], func=AF.Exp, scale=-math.log(10000.0) / 64.0), [a0])
            s.op('dve', lambda e: e.tensor_scalar(out=sgn[:], in0=pidx[:], scalar1=64.0, scalar2=2.0, op0=ALU.is_ge, op1=ALU.mult), [c2])
            a2 = s.op('dve', lambda e: e.tensor_scalar_add(out=sgn[:], in0=sgn[:], scalar1=-1.0), [s.last('dve')])
            wtok = None
            for ch in range(4):
                p0 = s.op('pool', lambda e, ch=ch: e.iota(posi[:], pattern=[[1, 2048]], base=ch * 2048, channel_multiplier=0),
                          [s.last('dve')])
                p1 = s.op('dve', lambda e: e.tensor_copy(out=posf[:], in_=posi[:]), [p0])
                p2 = s.op('dve', lambda e: e.tensor_scalar_mul(out=ang[:], in0=posf[:], scalar1=invf[:, 0:1]), [p1, a1, s.last('act')])
                def reduce_turns(dst, off):
                    s.op('dve', lambda e: e.tensor_scalar(out=dst[:], in0=ang[:], scalar1=1.0 / (2 * PI), scalar2=off, op0=ALU.mult, op1=ALU.add), [p2], seq=True)
                    s.op('dve', lambda e: e.tensor_copy(out=posi[:], in_=dst[:]), seq=True)
                    s.op('dve', lambda e: e.tensor_copy(out=posf[:], in_=posi[:]), seq=True)
                    s.op('dve', lambda e: e.tensor_tensor(out=dst[:], in0=dst[:], in1=posf[:], op=ALU.subtract), seq=True)
                    s.op('dve', lambda e: e.tensor_single_scalar(out=posf[:], in_=dst[:], scalar=0.5, op=ALU.is_gt), seq=True)
                    s.op('dve', lambda e: e.tensor_tensor(out=dst[:], in0=dst[:], in1=posf[:], op=ALU.subtract), seq=True)
                    s.op('dve', lambda e: e.tensor_single_scalar(out=posf[:], in_=dst[:], scalar=-0.5, op=ALU.is_lt), seq=True)
                    s.op('dve', lambda e: e.tensor_tensor(out=dst[:], in0=dst[:], in1=posf[:], op=ALU.add), seq=True)
                    return s.op('dve', lambda e: e.tensor_scalar_mul(out=dst[:], in0=dst[:], scalar1=2 * PI - 1e-5), seq=True)
                p3 = reduce_turns(r1, 0.0)
                p4 = reduce_turns(r2, 0.25)
                q1 = s.op('act', lambda e: e.activation(out=sn[:], in_=r1[:], func=AF.Sin), [p3, wtok])
                q2 = s.op('act', lambda e: e.activation(out=cs[:], in_=r2[:], func=AF.Sin), [p4, wtok])
                q3 = s.op('dve', lambda e: e.tensor_scalar_mul(out=sn[:], in0=sn[:], scalar1=sgn[:, 0:1]), [q1, a2])
                s.dma('sync', sinT[:, ch * 2048:(ch + 1) * 2048], sn[:], dsr, [q3])
                wtok = s.dma('sync', cosT[:, ch * 2048:(ch + 1) * 2048], cs[:], dsr, [q2])
            rot_tok = wtok
            s.fence([rot_tok])

        scr_tok = []
        with ExitStack() as es2:
            sb2 = lambda name, shape, dtype: es2.enter_context(nc.sbuf_tensor(name, shape, dtype))
            wsb = sb2("wsb", [128, 16, 2048], BF16)
            xst = [sb2("xst%d" % i, [128, DM], F32) for i in range(2)]
            hb = [sb2("hb%d" % i, [128, DM], BF16) for i in range(4)]
            hT = [sb2("hT%d" % i, [128, 16, 512], BF16) for i in range(2)]
            g1b = sb2("g1b", [128, DM], F32)
            ssq = sb2("ssq", [128, 4], F32)
            rstd = sb2("rstd", [128, 2], F32)
            cst = [sb2("cst%d" % i, [128, 512], F32) for i in range(2)]
            snt = [sb2("snt%d" % i, [128, 512], F32) for i in range(2)]
            qst = [sb2("qst%d" % i, [128, 512], BF16) for i in range(8)]
            vst = [sb2("vst%d" % i, [128, 512], BF16) for i in range(4)]
            rt1 = [sb2("rt1_%d" % i, [128, 512], F32) for i in range(2)]
            rt2 = [sb2("rt2_%d" % i, [128, 512], F32) for i in range(2)]
            dsx = [s.dsem() for _ in range(2)]
            dsw = [s.dsem() for _ in range(2)]
            dsg1 = s.dsem()
            dstab = [s.dsem() for _ in range(2)]
            dsq = [s.dsem() for _ in range(8)]
            dsv = [s.dsem() for _ in range(4)]
            gtok = s.dma('sync', g1b[:], g1.partition_broadcast(128), dsg1)
            ctk = [None, None]
            for kt in range(16):
                k = kt % 2
                ld = s.dma('sync', xst[k][:], win[kt * 128:(kt + 1) * 128, :], dsw[k], [ctk[k]])
                ce = 'dve' if k == 0 else 'pool'
                ctk[k] = s.op(ce, lambda e, k=k, kt=kt: e.tensor_copy(out=wsb[:, kt, :], in_=xst[k][:]), [ld])
            wtoks = list(ctk)
            xfree = list(ctk)
            hbfree = [None] * 4
            hh_tok = {}
            hTfree = [None, None]
            tpfree = [None] * 4
            qfree = [None] * 8
            vfree = [None] * 4
            hTready = [None, None]

            def prep_front(gi):
                for j in range(4):
                    i = gi * 4 + j
                    k = i % 2
                    k4 = i % 4
                    ld = s.dma('sync', xst[k][:], xb[i * 128:(i + 1) * 128, :], dsx[k], [xfree[k]])
                    sq = s.op('act', lambda e, k=k, k4=k4: e.activation(out=hb[k4][:], in_=xst[k][:], func=AF.Square,
                                                                       accum_out=ssq[:, k:k + 1]), [ld, hbfree[k4]])
                    r = rstd_chain(s, rstd[:, k:k + 1], ssq[:, k:k + 1], 1.0 / DM, [sq, xfree[k]])
                    hh = s.op('dve', lambda e, k=k, k4=k4: e.scalar_tensor_tensor(out=hb[k4][:], in0=xst[k][:], scalar=rstd[:, k:k + 1],
                                                                             in1=g1b[:], op0=ALU.mult, op1=ALU.mult), [r, gtok])
                    xfree[k] = hh
                    hh_tok[i] = hh

            def prep_back(gi):
                hsel = gi % 2
                evs = []
                for j in range(4):
                    i = gi * 4 + j
                    k4 = i % 4
                    hh = hh_tok.pop(i)
                    tp_ = j % 2
                    tpv = ps_all[:, tp_ * 1024:(tp_ + 1) * 1024].bitcast(BF16).rearrange("p (k t) -> p k t", t=128)
                    for kt in range(16):
                        last = s.op('pe', lambda e, k4=k4, kt=kt, tpv=tpv: e.transpose(out=tpv[:, kt, :], in_=hb[k4][:, kt * 128:(kt + 1) * 128],
                                                                                      identity=identb[:]),
                                    [hh, tpfree[2 * tp_], tpfree[2 * tp_ + 1], cst_tok], signal=(kt == 15))
                    hbfree[k4] = last
                    e0 = s.op('act', lambda e, j=j, tpv=tpv: e.copy(out=hT[hsel][:, 0:8, j * 128:(j + 1) * 128], in_=tpv[:, 0:8, :]),
                              [last, hTfree[hsel]])
                    e1 = s.op('dve', lambda e, j=j, tpv=tpv: e.tensor_copy(out=hT[hsel][:, 8:16, j * 128:(j + 1) * 128], in_=tpv[:, 8:16, :]),
                              [last, hTfree[hsel]])
                    tpfree[2 * tp_], tpfree[2 * tp_ + 1] = e0, e1
                    evs += [e0, e1]
                hTready[hsel] = evs

            mmbank = [4, 5, 6, 7]
            bankfree = {b: None for b in mmbank}
            bctr = [0]

            def nextbank():
                b = mmbank[bctr[0] % len(mmbank)]
                bctr[0] += 1
                return b

            def mm(gi):
                hsel = gi % 2
                h = hT[hsel]
                rdy = hTready[hsel]
                t0c = gi * 512
                tb = gi % 2
                tabt = [s.dma('sync', cst[tb][:], cosT[:, t0c:t0c + 512], dstab[tb], [rot_tok, tabfree[tb]]),
                        s.dma('sync', snt[tb][:], sinT[:, t0c:t0c + 512], dstab[tb], [rot_tok, tabfree[tb]])]
                tabt = [(dstab[tb].sem, dstab[tb].count)]
                lastmm = None

                def blockmm(blk):
                    b = nextbank()
                    for kt in range(16):
                        t = s.op('pe', lambda e, kt=kt, b=b, blk=blk: e.matmul(bank(b), lhsT=wsb[:, kt, blk * 128:(blk + 1) * 128],
                                                                               rhs=h[:, kt, :], start=(kt == 0), stop=(kt == 15)),
                                 rdy + wtoks + [bankfree[b]], signal=(kt == 15))
                    return b, t
                for blk in range(4):
                    b, t = blockmm(blk)
                    if blk < 2:
                        ev = s.op('act', lambda e, b=b, blk=blk: e.activation(out=qst[blk][:], in_=bank(b), func=AF.Copy, scale=SCALE),
                                  [t, qfree[blk]])
                    else:
                        ev = s.op('act', lambda e, b=b, blk=blk: e.copy(out=qst[blk][:], in_=bank(b)), [t, qfree[blk]])
                    bankfree[b] = ev
                    qfree[blk] = s.dma('sync', QK[blk, :, t0c:t0c + 512], qst[blk][:], dsq[blk], [ev])
                    lastmm = t
                for blk in range(4, 8):
                    ba, ta = blockmm(blk)
                    bb, tb_ = blockmm(blk + 4)
                    r = blk % 2
                    u1 = s.op('dve', lambda e, ba=ba, r=r: e.tensor_tensor(out=rt1[r][:], in0=bank(ba), in1=cst[tb][:], op=ALU.mult),
                              [ta] + tabt + [rtfree[r]])
                    bankfree[ba] = u1
                    u2 = s.op('dve', lambda e, bb=bb, r=r: e.tensor_tensor(out=rt2[r][:], in0=bank(bb), in1=snt[tb][:], op=ALU.mult),
                              [tb_] + tabt)
                    bankfree[bb] = u2
                    u3 = s.op('pool', lambda e, r=r, blk=blk: e.tensor_tensor(out=qst[blk][:], in0=rt1[r][:], in1=rt2[r][:], op=ALU.add),
                              [u1, u2, qfree[blk]])
                    rtfree[r] = u3
                    qfree[blk] = s.dma('sync', QK[blk, :, t0c:t0c + 512], qst[blk][:], dsq[blk], [u3])
                    lastmm = tb_
                tabfree[tb] = s.last('dve')
                for j in range(4):
                    b = nextbank()
                    for kt in range(16):
                        t = s.op('pe', lambda e, kt=kt, b=b, j=j: e.matmul(bank(b), lhsT=h[:, kt, j * 128:(j + 1) * 128],
                                                                           rhs=wsb[:, kt, 1536:2048], start=(kt == 0), stop=(kt == 15)),
                                 rdy + wtoks + [bankfree[b]], signal=(kt == 15))
                    ev = s.op('act', lambda e, b=b, j=j: e.copy(out=vst[j][:], in_=bank(b)), [t, vfree[j]])
                    bankfree[b] = ev
                    r0 = t0c + j * 128
                    vfree[j] = s.dma('sync', VV[r0:r0 + 128, :], vst[j][:], dsv[j], [ev])
                    lastmm = t
                hTfree[hsel] = lastmm

            tabfree = [None, None]
            rtfree = [None, None]
            NG = 16
            prep_front(0)
            prep_back(0)
            for gi in range(NG):
                if gi + 1 < NG:
                    prep_front(gi + 1)
                mm(gi)
                if gi + 1 < NG:
                    prep_back(gi + 1)
            scr_tok = [t for t in qfree + vfree if t is not None]
            s.fence(scr_tok)

        if STAGE <= 1:
            es_cv.close()
            return nc, finish(nc, s, es, y, scr_tok)

        mix_tok = []
        dsm = [s.dsem() for _ in range(4)]
        mixdone = lambda: [(d.sem, d.count) for d in dsm if d.count > 0]
        with ExitStack() as es2:
            sb2 = lambda name, shape, dtype: es2.enter_context(nc.sbuf_tensor(name, shape, dtype))
            qT = [sb2("qT%d" % i, [128, SEQ], BF16) for i in range(2)]
            kT = [sb2("kT%d" % i, [128, SEQ], BF16) for i in range(2)]
            vS = [sb2("vS%d" % i, [128, 64, 128], BF16) for i in range(2)]
            e_sb = [sb2("e_sb%d" % i, [128, 512], F32) for i in range(2)]
            Lp = [sb2("Lp%d" % i, [128, 512], BF16) for i in range(3)]
            Ls = [sb2("Ls%d" % i, [128, 512], BF16) for i in range(3)]
            A_sb = [sb2("A_sb%d" % i, [128, 512], BF16) for i in range(2)]
            o_sb = sb2("o_sb", [128, 512], F32)
            sq_sb = sb2("sq_sb", [128, 512], F32)
            rs_sb = sb2("rs_sb", [128, 512], F32)
            mo = [sb2("mo%d" % i, [128, 512], BF16) for i in range(2)]
            dsl = s.dsem()
            for hh in range(2):
                s.dma('sync', qT[hh][:], QK[hh], dsl)
                s.dma('sync', kT[hh][:], QK[2 + hh], dsl)
                s.dma('sync', vS[hh][:], VV[:, hh * 128:(hh + 1) * 128].rearrange("(n p) d -> p n d", p=128), dsl)
            ldtok = (dsl.sem, dsl.count)
            zfree = [None, None, None]
            ofree = [None, None]
            finfree = [None]
            efree = [None, None]
            lpfree = [None, None, None]
            lsfree = [None, None, None]
            asbfree = [None, None]
            mofree = [None, None]
            osbfree = [None]
            blocks = []
            for hh in range(2):
                for qt in range(16):
                    kbs = list(range(4 * qt + 3, -1, -1))
                    for n, kb in enumerate(kbs):
                        blocks.append(dict(h=hh, qt=qt, kb=kb, first=(n == 0), last=(n == len(kbs) - 1), diag=(kb - 4 * qt)))
            if STAGE == 2:
                blocks = [b for b in blocks if b['h'] == 0 and b['qt'] < 2]
            st = {}
            oidx = [0]
            lsw = [0]

            def sb_z(i):
                B = blocks[i]
                z = i % 3
                h, qt, kb = B['h'], B['qt'], B['kb']
                st.setdefault(i, {})
                st[i]['z'] = s.op('pe', lambda e: e.matmul(bank(z), lhsT=kT[h][:, kb * 128:(kb + 1) * 128],
                                                           rhs=qT[h][:, qt * 512:(qt + 1) * 512], start=True, stop=False),
                                  [ldtok, zfree[z]])

            def sb_act1(i):
                B = blocks[i]
                z = i % 3
                ez = i % 2
                l3 = i % 3
                t = s.op('act', lambda e: e.activation(out=e_sb[ez][:], in_=bank(z), func=AF.Exp), [st[i]['z'], efree[ez]])
                t2 = s.op('act', lambda e: e.activation(out=Lp[l3][:], in_=e_sb[ez][:], func=AF.Ln, bias=1.0), [t, lpfree[l3]])
                efree[ez] = t2
                if B['diag'] >= 0:
                    j = B['diag']
                    t2 = s.op('dve', lambda e: e.tensor_tensor(out=Lp[l3][:], in0=Lp[l3][:], in1=msb[:, j, :], op=ALU.mult), [t2, cst_tok])
                st[i]['lp'] = t2
                if not B['last']:
                    if B['first']:
                        nxt = (lsw[0] + 1) % 3
                        t3 = s.op('dve', lambda e: e.tensor_copy(out=Ls[nxt][:], in_=Lp[l3][:]), [t2, lsfree[nxt]])
                    else:
                        cur = st[i]['ls_in']
                        nxt = (cur + 1) % 3
                        t3 = s.op('dve', lambda e: e.tensor_tensor(out=Ls[nxt][:], in0=Ls[cur][:], in1=Lp[l3][:], op=ALU.add),
                                  [t2, lsfree[nxt], st[i]['ls_tok']])
                    lsw[0] = nxt
                    st.setdefault(i + 1, {})
                    st[i + 1]['ls_in'] = nxt
                    st[i + 1]['ls_tok'] = t3

            def sb_arg(i):
                B = blocks[i]
                z = i % 3
                l3 = i % 3
                lastflag = B['first']
                t = s.op('pe', lambda e: e.matmul(bank(z), lhsT=negU[:], rhs=Lp[l3][:], start=False, stop=lastflag),
                         [st[i]['lp'], cst_tok])
                if not B['first']:
                    cur = st[i]['ls_in']
                    t = s.op('pe', lambda e: e.matmul(bank(z), lhsT=negOnes[:], rhs=Ls[cur][:], start=False, stop=True),
                             [st[i]['ls_tok']])
                    lsfree[cur] = t
                lpfree[l3] = t
                st[i]['arg'] = t

            def sb_act2(i):
                B = blocks[i]
                z = i % 3
                a = i % 2
                t = s.op('act', lambda e: e.activation(out=A_sb[a][:], in_=bank(z), func=AF.Exp), [st[i]['arg'], asbfree[a]])
                zfree[z] = t
                if B['diag'] >= 0:
                    j = B['diag']
                    t = s.op('dve', lambda e: e.tensor_tensor(out=A_sb[a][:], in0=A_sb[a][:], in1=msb[:, j, :], op=ALU.mult), [t])
                st[i]['A'] = t

            def sb_av(i):
                B = blocks[i]
                a = i % 2
                h, qt, kb = B['h'], B['qt'], B['kb']
                if B['first']:
                    oidx[0] += 1
                ob = 4 + (oidx[0] % 2)
                t = s.op('pe', lambda e: e.matmul(bank(ob), lhsT=vS[h][:, kb, :], rhs=A_sb[a][:], start=B['first'], stop=B['last']),
                         [st[i]['A'], ofree[oidx[0] % 2] if B['first'] else None])
                asbfree[a] = t
                if B['last']:
                    sb_fin(h, qt, ob, oidx[0] % 2, t)
                del st[i]

            def sb_fin(h, qt, ob, oi, tok):
                c1 = s.op('act', lambda e: e.copy(out=o_sb[:], in_=bank(ob)), [tok, osbfree[0]])
                ofree[oi] = c1
                c2 = s.op('act', lambda e: e.activation(out=sq_sb[:], in_=o_sb[:], func=AF.Square), [c1, finfree[0]])
                m = s.op('pe', lambda e: e.matmul(bank(6), lhsT=onesf[:], rhs=sq_sb[:], start=True, stop=True), [c2, finfree[0], cst_tok])
                r = rstd_chain(s, rs_sb[:], bank(6), 1.0 / 128.0, [m, osbfree[0]])
                finfree[0] = r
                mi = (h * 16 + qt) % 2
                f = s.op('dve', lambda e: e.scalar_tensor_tensor(out=mo[mi][:], in0=o_sb[:], scalar=sbg_t[:, 0:1], in1=rs_sb[:],
                                                                 op0=ALU.mult, op1=ALU.mult), [r, c1, ptok, mofree[mi]])
                osbfree[0] = f
                mofree[mi] = s.dma('sync', mix_loc[qt, h * 128:(h + 1) * 128, :], mo[mi][:], dsm[mi], [f])
                mix_tok.append(mofree[mi])

            nb = len(blocks)
            for step in range(nb + 2):
                if step < nb:
                    sb_z(step)
                    sb_act1(step)
                if 1 <= step <= nb:
                    sb_arg(step - 1)
                    sb_act2(step - 1)
                if step >= 2:
                    sb_av(step - 2)
                if step % 10 == 5 and STAGE >= 4:
                    conv_step(1)
            wts_tok = conv_finish() if STAGE >= 4 else None
            s.fence(mixdone() + [t for t in out_tok if t is not None])

        es_cv.close()
        if STAGE <= 2:
            return nc, finish(nc, s, es, y, mixdone())

        with ExitStack() as es2:
            sb2 = lambda name, shape, dtype: es2.enter_context(nc.sbuf_tensor(name, shape, dtype))
            qD = [sb2("qD%d" % i, [128, SEQ], BF16) for i in range(2)]
            kD = [sb2("kD%d" % i, [128, SEQ], BF16) for i in range(2)]
            vD = sb2("vD", [128, 64, 256], BF16)
            E_sb = [sb2("E_sb%d" % i, [128, 512], BF16) for i in range(4)]
            Es = [[sb2("Es%d_%d" % (i, pp), [128, 512], F32) for pp in range(2)] for i in range(2)]
            rc = [sb2("rc%d" % i, [128, 512], F32) for i in range(2)]
            tA = sb2("tA", [128, 512], F32)
            tB = sb2("tB", [128, 512], F32)
            dh = [sb2("dh%d" % i, [128, 512], F32) for i in range(2)]
            sqd = sb2("sqd", [128, 512], F32)
            rsd = sb2("rsd", [128, 512], F32)
            mod_ = [sb2("mod%d" % i, [128, 512], BF16) for i in range(2)]
            dsl = s.dsem()
            for c in range(2):
                s.dma('sync', qD[c][:], QK[4 + c], dsl)
                s.dma('sync', kD[c][:], QK[6 + c], dsl)
            s.dma('sync', vD[:], VV[:, 256:512].rearrange("(n p) d -> p n d", p=128), dsl)
            ldtok = (dsl.sem, dsl.count)
            sfree = [None, None, None]
            Efree = [None] * 4
            Efree2 = [None] * 4
            obfree = [None] * 4
            esfree = [[None, None], [None, None]]
            finfree = [None]
            modfree = [None, None]
            dblocks = []
            for qt in range(16):
                kbs = list(range(0, 4 * qt + 4))
                for n, kb in enumerate(kbs):
                    dblocks.append(dict(qt=qt, kb=kb, first=(n == 0), last=(n == len(kbs) - 1), diag=(kb - 4 * qt)))
            if STAGE == 3:
                dblocks = [b for b in dblocks if b['qt'] < 2]
            std = {}
            sctr = [0]
            ectr = [0]

            def d_s(i):
                B = dblocks[i]
                std[i] = {}
                for c in range(2):
                    sbk = sctr[0] % 3
                    sctr[0] += 1
                    t = s.op('pe', lambda e, c=c, sbk=sbk: e.matmul(bank(sbk), lhsT=kD[c][:, B['kb'] * 128:(B['kb'] + 1) * 128],
                                                                   rhs=qD[c][:, B['qt'] * 512:(B['qt'] + 1) * 512], start=True, stop=True),
                             [ldtok, sfree[sbk]])
                    std[i][('s', c)] = (sbk, t)

            def d_act(i):
                B = dblocks[i]
                for c in range(2):
                    sbk, t = std[i][('s', c)]
                    ei = ectr[0] % 4
                    ectr[0] += 1
                    t2 = s.op('act', lambda e, sbk=sbk, ei=ei: e.activation(out=E_sb[ei][:], in_=bank(sbk), func=AF.Exp, scale=SCALE),
                              [t, Efree[ei], Efree2[ei]])
                    sfree[sbk] = t2
                    if B['diag'] >= 0:
                        j = B['diag']
                        t2 = s.op('dve', lambda e, ei=ei, j=j: e.tensor_tensor(out=E_sb[ei][:], in0=E_sb[ei][:], in1=mdf[:, j, :], op=ALU.mult),
                                  [t2, cst_tok])
                    aeng = 'dve' if c == 0 else 'pool'
                    pq = B['qt'] % 2
                    if B['first']:
                        t3 = s.op(aeng, lambda e, ei=ei, c=c, pq=pq: e.tensor_copy(out=Es[c][pq][:], in_=E_sb[ei][:]), [t2, esfree[c][pq]])
                    else:
                        t3 = s.op(aeng, lambda e, ei=ei, c=c, pq=pq: e.tensor_tensor(out=Es[c][pq][:], in0=Es[c][pq][:], in1=E_sb[ei][:], op=ALU.add),
                                  [t2, s.last(aeng)])
                    Efree2[ei] = t3
                    std[i][('E', c)] = (ei, t2, t3)

            def d_av(i):
                B = dblocks[i]
                lasts = []
                for c in range(2):
                    ei, t2, t3 = std[i][('E', c)]
                    for hf in range(2):
                        ob = 3 + 2 * c + hf
                        t = s.op('pe', lambda e, ob=ob, hf=hf, ei=ei: e.matmul(bank(ob), lhsT=vD[:, B['kb'], hf * 128:(hf + 1) * 128],
                                                                              rhs=E_sb[ei][:], start=B['first'], stop=B['last']),
                                 [t2, obfree[2 * c + hf] if B['first'] else None])
                    Efree[ei] = t
                    std[i][('p', c)] = t3
                    lasts.append((t, t3))
                if B['last']:
                    d_fin(B['qt'], lasts)
                del std[i]

            def d_fin(qt, lasts):
                for c in range(2):
                    pe_t, pool_t = lasts[c]
                    m = s.op('pe', lambda e, c=c: e.matmul(bank(7), lhsT=onesf[:], rhs=Es[c][qt % 2][:], start=True, stop=True),
                             [pool_t, finfree[0], cst_tok])
                    esfree[c][qt % 2] = m
                    r = s.op('dve', lambda e, c=c: e.reciprocal(out=rc[c][:], in_=bank(7)), [m])
                    finfree[0] = r
                lastpe = lasts[1][0]
                for hf in range(2):
                    a = s.op('dve', lambda e, hf=hf: e.tensor_tensor(out=tA[:], in0=bank(3 + hf), in1=rc[0][:], op=ALU.mult), [lastpe, s.last('dve')])
                    obfree[hf] = a
                    b_ = s.op('dve', lambda e, hf=hf: e.tensor_tensor(out=tB[:], in0=bank(5 + hf), in1=rc[1][:], op=ALU.mult), [lastpe, a])
                    obfree[2 + hf] = b_
                    s.op('dve', lambda e, hf=hf: e.scalar_tensor_tensor(out=dh[hf][:], in0=tB[:], scalar=neglam[:, 0:1], in1=tA[:],
                                                                        op0=ALU.mult, op1=ALU.add), [b_, s.last('pe')])
                dtok = s.last('dve')
                for hf in range(2):
                    c2 = s.op('act', lambda e, hf=hf: e.activation(out=sqd[:], in_=dh[hf][:], func=AF.Square), [dtok, s.last('pe')])
                    m = s.op('pe', lambda e, hf=hf: e.matmul(bank(7), lhsT=onesf[:], rhs=sqd[:], start=(hf == 0), stop=(hf == 1)),
                             [c2, finfree[0]])
                r = rstd_chain(s, rsd[:], bank(7), 1.0 / 256.0, [m, modfree[0], modfree[1]])
                finfree[0] = r
                for hf in range(2):
                    f = s.op('dve', lambda e, hf=hf: e.scalar_tensor_tensor(out=mod_[hf][:], in0=dh[hf][:], scalar=gsc[:, hf:hf + 1], in1=rsd[:],
                                                                            op0=ALU.mult, op1=ALU.mult), [r, modfree[hf]])
                    modfree[hf] = s.dma('sync', mix_loc[qt, 256 + hf * 128:256 + (hf + 1) * 128, :], mod_[hf][:], dsm[2 + hf], [f])
                    dq_tok.append(modfree[hf])
                if STAGE >= 4:
                    s._wait('pool', dq_tok[-2:] + mixdone())
                    nc.gpsimd.collective_compute("AllGather", ALU.bypass, replica_groups=[[0, 1, 2, 3], [4, 5, 6, 7]],
                                                 ins=[mix_loc[qt]], outs=[mix_all[qt]]).then_inc(cc_sem)
                    cc_cnt[0] += 1

            nb = len(dblocks)
            dq_tok = []
            for step in range(nb + 1):
                if step < nb:
                    d_s(step)
                    d_act(step)
                if step >= 1:
                    d_av(step - 1)
            s.fence(mixdone())

        if STAGE <= 3:
            return nc, finish(nc, s, es, y, mixdone())

        mixall_tok = (cc_sem, cc_cnt[0])
        if STAGE == 35:
            return nc, finish(nc, s, es, y, mixdone() + [mixall_tok])

        comb = sbp("comb", [128, 16, 32], F32)
        c1_toks = []
        with ExitStack() as es2:
            sb2 = lambda name, shape, dtype: es2.enter_context(nc.sbuf_tensor(name, shape, dtype))
            mixT = sb2("mixT", [128, 16, NTOK], BF16)
            wo = sb2("wo", [128, 16, DM], BF16)
            g2b = sb2("g2b", [128, DM], F32)
            wr_sb = sb2("wr_sb", [128, 16, 36], F32)
            brb = sb2("brb", [128, 36], F32)
            selb = sb2("selb", [128, 4], F32)
            xs_t = [sb2("xs_t0", [128, DM], F32)] * 2
            x1 = [sb2("x1_0", [128, DM], F32)] * 2
            h2 = [sb2("h2_0", [128, DM], F32)] * 2
            wst = h2
            jk = sb2("jk", [128, DM], BF16)
            h2lo = sb2("h2lo", [128, DM], BF16)
            h2Tl = sb2("h2Tl", [128, 16, 128], BF16)
            wr_hi = sb2("wr_hi", [128, 16, 36], BF16)
            wr_lo = sb2("wr_lo", [128, 16, 36], BF16)
            h2Tb = [sb2("h2Tb%d" % i, [128, 16, 128], BF16) for i in range(2)]
            ssq2 = sb2("ssq2", [128, 2], F32)
            rstd2 = sb2("rstd2", [128, 2], F32)
            lg = sb2("lg", [128, 36], F32)
            rt = sb2("rt", [128, 64], F32)
            dsA = s.dsem()
            dsW = [s.dsem()] * 2
            dsG = [s.dsem() for _ in range(2)]
            dsX = [s.dsem()] * 2
            dsX1 = [s.dsem()] * 2
            dsH = [s.dsem() for _ in range(2)]
            s.dma('sync', g2b[:], g2.partition_broadcast(128), dsA)
            s.dma('sync', wr_sb[:], wr.rearrange("(k p) c -> p k c", p=128), dsA)
            s.dma('sync', brb[:], br.partition_broadcast(128), dsA)
            s.dma('sync', selb[:], sel.partition_broadcast(128), dsA)
            atok = (dsA.sem, dsA.count)
            w1 = s.op('dve', lambda e: e.tensor_copy(out=wr_hi[:], in_=wr_sb[:]), [atok])
            wrtok = s.op('dve', lambda e: e.tensor_tensor(out=wr_lo[:], in0=wr_sb[:], in1=wr_hi[:], op=ALU.subtract), [w1])
            jkfree = [None]
            ctk = [None, None]
            for kt in range(16):
                k = kt % 2
                ld = s.dma('sync', wst[k][:], wout[kt * 128:(kt + 1) * 128, :], dsW[k], [ctk[0], ctk[1]])
                ce = 'dve' if k == 0 else 'pool'
                ctk[k] = s.op(ce, lambda e, k=k, kt=kt: e.tensor_copy(out=wo[:, kt, :], in_=wst[k][:]), [ld])
            wotoks = list(ctk)
            gt = None
            selbuf = [jk, h2lo]
            bufree = [None, None]
            nsel = 0
            for kt in range(16):
                for j in range(4):
                    sbuf_ = selbuf[nsel % 2]
                    ld = s.dma('sync', sbuf_[:].rearrange("p (q t) -> p q t", q=4),
                               mix_all[4 * j:4 * j + 4, kt * 128:(kt + 1) * 128, :].rearrange("q p t -> p q t"), dsG[nsel % 2],
                               [mixall_tok, bufree[nsel % 2]])
                    if j == 0:
                        gt = s.op('dve', lambda e, kt=kt, sbuf_=sbuf_: e.tensor_scalar_mul(out=mixT[:, kt, :], in0=sbuf_[:], scalar1=selb[:, 0:1]), [ld, atok])
                    else:
                        gt = s.op('dve', lambda e, kt=kt, j=j, sbuf_=sbuf_: e.scalar_tensor_tensor(out=mixT[:, kt, :], in0=sbuf_[:], scalar=selb[:, j:j + 1],
                                                                                              in1=mixT[:, kt, :], op0=ALU.mult, op1=ALU.add), [ld, gt])
                    bufree[nsel % 2] = gt
                    nsel += 1
            gtok = gt
            xfree = _One([None])
            x1free = _One([None])
            h2free = _One([None])
            hbfree = [None, None]
            psfree = [None]
            tpfree = [None]
            tpfree2 = [None]
            h32free = [None]
            for tt in range(C1T):
                k = tt % 2
                ld = s.dma('sync', xs_t[k][:], xs[tt * 128:(tt + 1) * 128, :], dsX[k], [xfree[k]])
                for c in range(4):
                    for kt in range(16):
                        t = s.op('pe', lambda e, c=c, kt=kt: e.matmul(bank(c), lhsT=mixT[:, kt, tt * 128:(tt + 1) * 128],
                                                                      rhs=wo[:, kt, c * 512:(c + 1) * 512], start=(kt == 0), stop=(kt == 15)),
                                 [gtok] + wotoks + [psfree[0]], signal=(c == 3 and kt == 15))
                a = s.op('dve', lambda e, k=k: e.tensor_tensor(out=x1[k][:], in0=ps_all[:, 0:2048], in1=xs_t[k][:], op=ALU.add),
                         [t, ld, x1free[k]])
                psfree[0] = a
                xfree[k] = a
                st1 = s.dma('sync', X1[tt * 128:(tt + 1) * 128, :], x1[k][:], dsX1[k], [a])
                sq = s.op('act', lambda e, k=k: e.activation(out=jk[:], in_=x1[k][:], func=AF.Square, accum_out=ssq2[:, k:k + 1]), [a, jkfree[0]])
                r = rstd_chain(s, rstd2[:, k:k + 1], ssq2[:, k:k + 1], 1.0 / DM, [sq, h2free[k]])
                hh = s.op('dve', lambda e, k=k: e.scalar_tensor_tensor(out=h2[k][:], in0=x1[k][:], scalar=rstd2[:, k:k + 1], in1=g2b[:],
                                                                       op0=ALU.mult, op1=ALU.mult), [r, atok, h2free[k]])
                x1free[k] = st1
                xfree[k] = hh
                if C1R == 0:
                    c1_toks.append(st1)
                    continue
                hi_t = s.op('act', lambda e, k=k: e.copy(out=jk[:], in_=h2[k][:]), [hh, jkfree[0]])
                lo_t = s.op('dve', lambda e, k=k: e.tensor_tensor(out=h2lo[:], in0=h2[k][:], in1=jk[:], op=ALU.subtract), [hi_t, jkfree[0]])
                h2free[k] = lo_t
                tpv = ps_all[:, 2048:4096].bitcast(BF16).rearrange("p (k t) -> p k t", t=128)
                for kt in range(16):
                    s.op('pe', lambda e, kt=kt: e.transpose(out=tpv[:, kt, :], in_=jk[:, kt * 128:(kt + 1) * 128], identity=identb[:]),
                         [hi_t, tpfree[0], tpfree2[0], cst_tok], signal=False)
                for kt in range(16):
                    tp = s.op('pe', lambda e, kt=kt: e.transpose(out=tpv[:, 16 + kt, :], in_=h2lo[:, kt * 128:(kt + 1) * 128], identity=identb[:]),
                              [lo_t], signal=(kt == 15))
                jkfree[0] = tp
                e1 = s.op('dve', lambda e, k=k: e.tensor_copy(out=h2Tb[k][:], in_=tpv[:, 0:16, :]), [tp, hbfree[k], h32free[0]])
                e0 = s.op('act', lambda e: e.copy(out=h2Tl[:], in_=tpv[:, 16:32, :]), [tp, h32free[0]])
                tpfree[0] = e1
                tpfree2[0] = e0
                if os.environ.get('MK_NOH2T'):
                    hbfree[k] = e1
                else:
                    hbfree[k] = s.dma('sync', H2T.rearrange("(k p) t -> p k t", p=128)[:, :, tt * 128:(tt + 1) * 128], h2Tb[k][:], dsH[k], [e1])
                c1_toks.append(hbfree[k])
                c1_toks.append(st1)
                if C1R == 1:
                    continue
                combos = [(h2Tb[k], wr_hi), (h2Tl, wr_hi), (h2Tb[k], wr_lo)]
                for ci, (lh, rw) in enumerate(combos):
                    for kt in range(16):
                        lt = s.op('pe', lambda e, kt=kt, lh=lh, rw=rw, ci=ci: e.matmul(ps_all[:, 0:36], lhsT=lh[:, kt, :], rhs=rw[:, kt, :],
                                                                                   start=(ci == 0 and kt == 0), stop=(ci == 2 and kt == 15)),
                                  [e0, e1, a, wrtok], signal=(ci == 2 and kt == 15))
                h32free[0] = lt
                lgt = s.op('dve', lambda e: e.tensor_tensor(out=lg[:], in0=ps_all[:, 0:36], in1=brb[:], op=ALU.add), [lt])
                psfree[0] = lgt
                V = nc.vector
                cmb = comb[:, tt, :]
                s.op('dve', lambda e: e.reduce_max(out=rt[:, 0:1], in_=lg[:, 0:4], axis=AX.X), [lgt], seq=True)
                s.op('dve', lambda e: e.tensor_scalar(out=rt[:, 8:12], in0=lg[:, 0:4], scalar1=rt[:, 0:1], scalar2=None, op0=ALU.is_equal), seq=True)
                s.op('dve', lambda e: e.tensor_scalar(out=rt[:, 1:5], in0=lg[:, 0:4], scalar1=rt[:, 0:1], scalar2=None, op0=ALU.subtract), seq=True)
                d1 = s.op('dve', lambda e: e.tensor_scalar_mul(out=rt[:, 16:24], in0=lg[:, 4:12], scalar1=rt[:, 8:9]), seq=True)
                ex = s.op('act', lambda e: e.activation(out=rt[:, 1:5], in_=rt[:, 1:5], func=AF.Exp), [d1])
                for gq in range(1, 4):
                    s.op('dve', lambda e, gq=gq: e.scalar_tensor_tensor(out=rt[:, 16:24], in0=lg[:, 4 + 8 * gq:12 + 8 * gq], scalar=rt[:, 8 + gq:9 + gq],
                                                                        in1=rt[:, 16:24], op0=ALU.mult, op1=ALU.add), seq=True)
                s.op('dve', lambda e: e.reduce_max(out=rt[:, 24:25], in_=rt[:, 16:24], axis=AX.X), seq=True)
                d2 = s.op('dve', lambda e: e.tensor_scalar(out=rt[:, 32:40], in0=rt[:, 16:24], scalar1=rt[:, 24:25], scalar2=None, op0=ALU.subtract), seq=True)
                ex2 = s.op('act', lambda e: e.activation(out=rt[:, 32:40], in_=rt[:, 32:40], func=AF.Exp), [d2])
                s.op('dve', lambda e: e.reduce_sum(out=rt[:, 5:6], in_=rt[:, 1:5], axis=AX.X), [ex, ex2], seq=True)
                s.op('dve', lambda e: e.reciprocal(out=rt[:, 6:7], in_=rt[:, 5:6]), seq=True)
                s.op('dve', lambda e: e.reduce_max(out=rt[:, 40:41], in_=rt[:, 32:40], axis=AX.X), seq=True)
                s.op('dve', lambda e: e.tensor_scalar(out=rt[:, 41:49], in0=rt[:, 32:40], scalar1=rt[:, 40:41], scalar2=None, op0=ALU.is_equal), seq=True)
                s.op('dve', lambda e: e.scalar_tensor_tensor(out=rt[:, 49:57], in0=rt[:, 41:49], scalar=-4.0, in1=rt[:, 32:40],
                                                             op0=ALU.mult, op1=ALU.add), seq=True)
                s.op('dve', lambda e: e.reduce_max(out=rt[:, 57:58], in_=rt[:, 49:57], axis=AX.X), seq=True)
                s.op('dve', lambda e: e.tensor_tensor(out=rt[:, 58:59], in0=rt[:, 40:41], in1=rt[:, 57:58], op=ALU.add), seq=True)
                s.op('dve', lambda e: e.reciprocal(out=rt[:, 58:59], in_=rt[:, 58:59]), seq=True)
                s.op('dve', lambda e: e.tensor_tensor(out=rt[:, 58:59], in0=rt[:, 58:59], in1=rt[:, 6:7], op=ALU.mult), seq=True)
                s.op('dve', lambda e: e.tensor_tensor(out=rt[:, 59:60], in0=rt[:, 40:41], in1=rt[:, 58:59], op=ALU.mult), seq=True)
                s.op('dve', lambda e: e.tensor_tensor(out=rt[:, 60:61], in0=rt[:, 57:58], in1=rt[:, 58:59], op=ALU.mult), seq=True)
                s.op('dve', lambda e: e.tensor_scalar(out=rt[:, 49:57], in0=rt[:, 49:57], scalar1=rt[:, 57:58], scalar2=rt[:, 60:61],
                                                      op0=ALU.is_equal, op1=ALU.mult), seq=True)
                s.op('dve', lambda e: e.scalar_tensor_tensor(out=rt[:, 49:57], in0=rt[:, 41:49], scalar=rt[:, 59:60], in1=rt[:, 49:57],
                                                             op0=ALU.mult, op1=ALU.add), seq=True)
                for gq in range(4):
                    s.op('dve', lambda e, gq=gq: e.tensor_scalar_mul(out=comb[:, tt, gq * 8:(gq + 1) * 8], in0=rt[:, 49:57],
                                                                     scalar1=rt[:, 8 + gq:9 + gq]), seq=True)
            comb_tok = s.last('dve')
            s.fence(c1_toks + [comb_tok])

        if STAGE <= 4:
            return nc, finish(nc, s, es, y, c1_toks)

        with ExitStack() as es2:
            sb2 = lambda name, shape, dtype: es2.enter_context(nc.sbuf_tensor(name, shape, dtype))
            g3b = sb2("g3b", [128, DM], F32)
            dsg3 = s.dsem()
            g3tok = s.dma('sync', g3b[:], g3.partition_broadcast(128), dsg3)
            hg = [sb2("hg0", [128, 16, 512], BF16)] * 2
            wgu = [sb2("wgu%d" % i, [128, 2, 16, 512], BF16) for i in range(2)]
            wdd = [sb2("wdd%d" % i, [128, 4, DM], BF16) for i in range(2)]
            acc = [sb2("acc%d" % i, [128, DM], F32) for i in range(4)]
            sg = [sb2("sg%d" % i, [128, 512], F32) for i in range(2)]
            hTe = [sb2("hTe%d" % i, [128, 4, 512], BF16) for i in range(2)]
            x1t = [sb2("x1t0", [128, DM], F32)] * 2
            yo = [sb2("yo0", [128, DM], F32)] * 2
            jk2 = sb2("jk2", [128, DM], BF16)
            ssq3 = sb2("ssq3", [128, 2], F32)
            rstd3 = sb2("rstd3", [128, 2], F32)
            dsHg = [s.dsem()] * 2
            dsWt = [s.dsem() for _ in range(2)]
            dsXl = [s.dsem()] * 2
            dsY = [s.dsem()] * 2
            NGRP = 4
            NEXP = NE if STAGE >= 6 else 2
            wfree = [None, None]
            hgfree = _One([None])
            hTefree = [None, None]
            sgfree = [None, None]
            accfree = [None] * 4
            x1tfree = _One([None])
            yofree = _One([None])
            gubank = [0, 1, 2, 3]
            gufree = [None] * 4
            dnfree = [None] * 4
            H2Tv = H2T.rearrange("(k p) t -> p k t", p=128)
            ectr = 0
            yt = []
            for grp in range(NGRP):
                hs = grp % 2
                hl = s.dma('sync', hg[hs][:], H2Tv[:, :, grp * 512:(grp + 1) * 512], dsHg[hs], [hgfree[hs]] + c1_toks[-4:])
                lastpe_grp = None
                for ex in range(NEXP):
                    wsel = ectr % 2
                    ectr += 1
                    rk, il = ex // 8, ex % 8
                    for m in range(2):
                        for hq in range(2):
                            chk = il * 4 + m * 2 + hq
                            s.dma('sync', wgu[wsel][:, m, hq * 8:(hq + 1) * 8, :],
                                  Wgu_all[chk, rk * 1024:(rk + 1) * 1024, :].rearrange("(k p) c -> p k c", p=128), dsWt[wsel],
                                  [wfree[wsel], wts_tok])
                    for hq in range(2):
                        chk = il * 2 + hq
                        s.dma('sync', wdd[wsel][:, hq * 2:(hq + 1) * 2, :],
                              Wd_all[chk, rk * 256:(rk + 1) * 256, :].rearrange("(k p) c -> p k c", p=128), dsWt[wsel],
                              [wfree[wsel], wts_tok])
                    wl = (dsWt[wsel].sem, dsWt[wsel].count)
                    hsel = ex % 2
                    for jt in range(4):
                        bg = (2 * jt) % 4
                        bu = (2 * jt + 1) % 4
                        for m, b in ((0, bg), (1, bu)):
                            for kt in range(16):
                                t = s.op('pe', lambda e, m=m, b=b, kt=kt, jt=jt: e.matmul(bank(b), lhsT=wgu[wsel][:, m, kt, jt * 128:(jt + 1) * 128],
                                                                                         rhs=hg[hs][:, kt, :], start=(kt == 0), stop=(kt == 15)),
                                         [wl, hl, gufree[b]], signal=(kt == 15))
                            if m == 0:
                                tg = t
                            else:
                                tu = t
                        si = jt % 2
                        a1 = s.op('act', lambda e, bg=bg, si=si: e.activation(out=sg[si][:], in_=bank(bg), func=AF.Silu), [tg, sgfree[si]])
                        gufree[bg] = a1
                        a2 = s.op('dve', lambda e, bu=bu, si=si, jt=jt: e.tensor_tensor(out=hTe[hsel][:, jt, :], in0=bank(bu), in1=sg[si][:], op=ALU.mult),
                                  [tu, a1, hTefree[hsel]])
                        gufree[bu] = a2
                        sgfree[si] = a2
                    hready = a2
                    for tl in range(4):
                        tt = grp * 4 + tl
                        for c in range(4):
                            b = 4 + c
                            for jt in range(4):
                                t = s.op('pe', lambda e, b=b, jt=jt, tl=tl, c=c: e.matmul(bank(b), lhsT=hTe[hsel][:, jt, tl * 128:(tl + 1) * 128],
                                                                                         rhs=wdd[wsel][:, jt, c * 512:(c + 1) * 512], start=(jt == 0), stop=(jt == 3)),
                                         [hready, dnfree[c]], signal=(jt == 3))
                            eidx = ex
                            if ex == 0:
                                u = s.op('dve', lambda e, b=b, tl=tl, c=c, tt=tt, eidx=eidx: e.tensor_scalar_mul(
                                    out=acc[tl][:, c * 512:(c + 1) * 512], in0=bank(b), scalar1=comb[:, tt, eidx:eidx + 1]), [t, accfree[tl], comb_tok])
                            else:
                                u = s.op('dve', lambda e, b=b, tl=tl, c=c, tt=tt, eidx=eidx: e.scalar_tensor_tensor(
                                    out=acc[tl][:, c * 512:(c + 1) * 512], in0=bank(b), scalar=comb[:, tt, eidx:eidx + 1],
                                    in1=acc[tl][:, c * 512:(c + 1) * 512], op0=ALU.mult, op1=ALU.add), [t])
                            dnfree[c] = u
                        lastpe_grp = t
                    wfree[wsel] = lastpe_grp
                    hTefree[hsel] = lastpe_grp
                hgfree[hs] = lastpe_grp
                for tl in range(4):
                    tt = grp * 4 + tl
                    k = tt % 2
                    ld = s.dma('sync', x1t[k][:], X1[tt * 128:(tt + 1) * 128, :], dsXl[k], [x1tfree[k]] + c1_toks)
                    a = s.op('dve', lambda e, k=k, tl=tl: e.tensor_tensor(out=x1t[k][:], in0=x1t[k][:], in1=acc[tl][:], op=ALU.add), [ld, s.last('dve')])
                    accfree[tl] = a
                    sq = s.op('act', lambda e, k=k: e.activation(out=jk2[:], in_=x1t[k][:], func=AF.Square, accum_out=ssq3[:, k:k + 1]), [a])
                    r = rstd_chain(s, rstd3[:, k:k + 1], ssq3[:, k:k + 1], 1.0 / DM, [sq, yofree[k]])
                    f = s.op('dve', lambda e, k=k: e.scalar_tensor_tensor(out=yo[k][:], in0=x1t[k][:], scalar=rstd3[:, k:k + 1], in1=g3b[:],
                                                                          op0=ALU.mult, op1=ALU.mult), [r, g3tok, yofree[k]])
                    x1tfree[k] = f
                    yofree[k] = s.dma('sync', y[tt * 128:(tt + 1) * 128, :], yo[k][:], dsY[k], [f])
                    yt.append(yofree[k])
            return nc, finish(nc, s, es, y, yt)


def finish(nc, s, es, y, toks):
    print('SCHED counts', s.cnt, 'ndsem', s.nds, flush=True)
    s.fence(toks)
    return None


_CACHE = {}


def kernel(**inputs):
    x = np.ascontiguousarray(inputs["x"], dtype=np.float32)
    w_in = np.asarray(inputs["w_in"], dtype=np.float32)[0]
    w_out = np.asarray(inputs["w_out"], dtype=np.float32)[0]
    wg = np.asarray(inputs["w_gate"], dtype=np.float32)[0]
    wu = np.asarray(inputs["w_up"], dtype=np.float32)[0]
    wd = np.asarray(inputs["w_down"], dtype=np.float32)[0]
    f = lambda k: np.asarray(inputs[k], dtype=np.float32)
    if "nc" not in _CACHE:
        _CACHE["nc"] = build_program()[0]
    nc = _CACHE["nc"]
    wr = np.ascontiguousarray(np.concatenate([f("w_group_router")[0], f("w_expert_router")[0]], axis=1))
    br = np.ascontiguousarray(np.concatenate([f("b_group_router")[0], f("b_expert_router")[0]], axis=0)[None, :])
    lam4 = np.ascontiguousarray(np.stack([f("diff_lambda_q1")[0], f("diff_lambda_k1")[0], f("diff_lambda_q2")[0], f("diff_lambda_k2")[0]], 0))
    rows = []
    for i in range(4):
        rows += [np.arange(2 * i * 128, 2 * i * 128 + 256), np.arange(1024 + i * 256, 1024 + i * 256 + 256)]
    wout_p = np.ascontiguousarray(w_out[np.concatenate(rows), :])
    sw = np.concatenate([np.arange(64, 128), np.arange(0, 64)])
    in_maps = []
    for c in range(8):
        b, g = c // 4, c % 4
        cols = []
        for h in (2 * g, 2 * g + 1):
            cols.append(np.arange(h * 128, h * 128 + 128))
        for h in (2 * g, 2 * g + 1):
            cols.append(1024 + np.arange(h * 128, h * 128 + 128))
        dq0 = 3072 + g * 256
        dk0 = 4096 + g * 256
        for base in (dq0, dq0 + 128, dk0, dk0 + 128):
            cols.append(base + np.arange(128))
        for base in (dq0, dq0 + 128, dk0, dk0 + 128):
            cols.append(base + sw)
        for h in (2 * g, 2 * g + 1):
            cols.append(2048 + np.arange(h * 128, h * 128 + 128))
        cols.append(5120 + g * 256 + np.arange(256))
        cols = np.concatenate(cols)
        assert cols.size == 2048
        dsg = f("diff_subln_gain")[0].reshape(2, 128).T
        gix = ((np.arange(16)[None, :] * 128 + np.arange(128)[:, None]) * 4 + g).astype(np.int32)
        in_maps.append({
            "xb": x[b],
            "xs": np.ascontiguousarray(x[b, g * NTOK:(g + 1) * NTOK]),
            "win": np.ascontiguousarray(w_in[:, cols]),
            "g1": f("attn_norm_gain"), "g2": f("ffn_norm_gain"), "g3": f("final_norm_gain")[None, :],
            "sbg": np.ascontiguousarray(f("sb_norm_gain")[0][:, None]),
            "dsg": np.ascontiguousarray(dsg),
            "lam4": lam4, "wout": wout_p, "wr": wr, "br": br,
            "wgm": np.ascontiguousarray(wg[8 * g:8 * g + 8].reshape(8 * DM, DE)),
            "wum": np.ascontiguousarray(wu[8 * g:8 * g + 8].reshape(8 * DM, DE)),
            "wdm": np.ascontiguousarray(wd[8 * g:8 * g + 8].reshape(8 * DE, DM)),
            "sel": np.eye(4, dtype=np.float32)[g:g + 1].copy(),
        })
    res = run_bass_kernel_spmd(nc, in_maps, core_ids=list(range(8)))
    _CACHE["res"] = res
    out = np.empty((2, SEQ, DM), np.float32)
    for c in range(8):
        b, g = c // 4, c % 4
        out[b, g * NTOK:(g + 1) * NTOK] = res.results[c]["y"]
    return out
```

```python
import os
import math
import numpy as np
from contextlib import ExitStack
import concourse.bass as bass
import concourse.mybir as mybir
from concourse.bass_utils import run_bass_kernel_spmd

F32 = mybir.dt.float32
BF16 = mybir.dt.bfloat16
I32 = mybir.dt.int32
AF = mybir.ActivationFunctionType
ALU = mybir.AluOpType
AX = mybir.AxisListType

SEQ = 8192
DM = 2048
NTOK = 2048
NE = 32
DE = 512
EPS = 1e-6
SCALE = 1.0 / math.sqrt(128.0)
LAM_INIT = 0.8 - 0.6 * math.exp(0.0)
PI = math.pi

STAGE = int(os.environ.get("MK_STAGE", "99"))
C1T = int(os.environ.get("MK_C1T", "16"))
C1R = int(os.environ.get("MK_C1R", "2"))


class _One(list):
    def __getitem__(self, i):
        return list.__getitem__(self, 0)

    def __setitem__(self, i, v):
        list.__setitem__(self, 0, v)


class DS:
    def __init__(self, sem):
        self.sem = sem
        self.count = 0


class Sched:
    def __init__(self, nc, es):
        self.nc = nc
        self.es = es
        self.eng = {'sync': nc.sync, 'act': nc.scalar, 'pe': nc.tensor, 'dve': nc.vector, 'pool': nc.gpsimd}
        self.sem = {k: es.enter_context(nc.semaphore("cs_" + k)) for k in ['act', 'pe', 'dve', 'pool']}
        self.cnt = {k: 0 for k in self.sem}
        self.seen = {}
        self.nds = 0
        self.pending = []

    def dsem(self):
        self.nds += 1
        return DS(self.es.enter_context(self.nc.semaphore("ds%d" % self.nds)))

    def _wait(self, eng, deps):
        need = {}
        for d in deps:
            if d is None:
                continue
            sem, val = d
            if val > need.get(sem, (None, 0))[1]:
                need[sem] = (sem, val)
        for sem, val in need.values():
            if self.seen.get((eng, sem), 0) >= val:
                continue
            self.eng[eng].wait_ge(sem, val)
            self.seen[(eng, sem)] = val

    def op(self, eng, fn, deps=(), signal=True, seq=False):
        deps = list(deps)
        if seq and self.cnt[eng] > 0:
            deps.append((self.sem[eng], self.cnt[eng]))
        self._wait(eng, deps)
        ins = fn(self.eng[eng])
        if signal or eng != 'pe':
            self.cnt[eng] += 1
            ins.then_inc(self.sem[eng], 1)
            return (self.sem[eng], self.cnt[eng])
        return None

    def dma(self, q, out, in_, ds, deps=()):
        self._wait(q, deps)
        ds.count += 16
        self.eng[q].dma_start(out=out, in_=in_).then_inc(ds.sem, 16)
        tok = (ds.sem, ds.count)
        return tok

    def last(self, eng):
        return (self.sem[eng], self.cnt[eng])

    def fence(self, extra=()):
        toks = [self.last(k) for k in self.sem] + list(extra)
        for e in ['sync', 'act', 'pe', 'dve', 'pool']:
            self._wait(e, toks)


def rstd_chain(s, dst, src, inv_n, deps):
    t1 = s.op('dve', lambda e: e.tensor_scalar(out=dst, in0=src, scalar1=inv_n, scalar2=EPS, op0=ALU.mult, op1=ALU.add), deps)
    t2 = s.op('act', lambda e: e.activation(out=dst, in_=dst, func=AF.Ln), [t1])
    t3 = s.op('act', lambda e: e.activation(out=dst, in_=dst, func=AF.Exp, scale=-0.5), [t2])
    return t3


def build_program():
    nc = bass.Bass("TRN2", target_bir_lowering=False)
    dt = lambda name, shape, dtype, kind: nc.dram_tensor(name, shape, dtype, kind=kind).ap()
    xb = dt("xb", [SEQ, DM], F32, "ExternalInput")
    xs = dt("xs", [NTOK, DM], F32, "ExternalInput")
    win = dt("win", [DM, 2048], F32, "ExternalInput")
    g1 = dt("g1", [1, DM], F32, "ExternalInput")
    g2 = dt("g2", [1, DM], F32, "ExternalInput")
    g3 = dt("g3", [1, DM], F32, "ExternalInput")
    sbg = dt("sbg", [128, 1], F32, "ExternalInput")
    dsg = dt("dsg", [128, 2], F32, "ExternalInput")
    lam4 = dt("lam4", [4, 128], F32, "ExternalInput")
    wout = dt("wout", [DM, DM], F32, "ExternalInput")
    wr = dt("wr", [DM, 36], F32, "ExternalInput")
    br = dt("br", [1, 36], F32, "ExternalInput")
    wgm = dt("wgm", [8 * DM, DE], F32, "ExternalInput")
    wum = dt("wum", [8 * DM, DE], F32, "ExternalInput")
    wdm = dt("wdm", [8 * DE, DM], F32, "ExternalInput")
    sel = dt("sel", [1, 4], F32, "ExternalInput")
    y = dt("y", [NTOK, DM], F32, "ExternalOutput")
    cosT = dt("cosT", [128, SEQ], F32, "Internal")
    sinT = dt("sinT", [128, SEQ], F32, "Internal")
    DBG = "ExternalOutput" if STAGE <= 3 else "Internal"
    DBG2 = "ExternalOutput" if STAGE <= 4 else "Internal"
    QK = dt("QK", [8, 128, SEQ], BF16, DBG)
    VV = dt("VV", [SEQ, 512], BF16, DBG)
    mix_loc = dt("mix_loc", [16, 512, 512], BF16, DBG)
    mix_all = dt("mix_all", [16, 2048, 512], BF16, "Internal")
    X1 = dt("X1", [NTOK, DM], F32, DBG2)
    H2T = dt("H2T", [DM, NTOK], BF16, DBG2)
    Wgu_loc = dt("Wgu_loc", [32, 1024, DE], BF16, "Internal")
    Wgu_all = dt("Wgu_all", [32, 4096, DE], BF16, "Internal")
    Wd_loc = dt("Wd_loc", [16, 256, DM], BF16, "Internal")
    Wd_all = dt("Wd_all", [16, 1024, DM], BF16, "Internal")

    with ExitStack() as es:
        s = Sched(nc, es)
        sbp = lambda name, shape, dtype: es.enter_context(nc.sbuf_tensor(name, shape, dtype))
        ps_all = es.enter_context(nc.psum_tensor("ps_all", [128, 8 * 512], F32))
        bank = lambda i: ps_all[:, i * 512:(i + 1) * 512]
        cc_sem = es.enter_context(nc.semaphore("cc_sem"))
        cc_cnt = [0]

        pidx = sbp("pidx", [128, 1], F32)
        identf = sbp("identf", [128, 128], F32)
        identb = sbp("identb", [128, 128], BF16)
        negU = sbp("negU", [128, 128], BF16)
        negOnes = sbp("negOnes", [128, 128], BF16)
        onesf = sbp("onesf", [128, 128], F32)
        msb = sbp("msb", [128, 4, 512], BF16)
        mdf = sbp("mdf", [128, 4, 512], BF16)
        thr = sbp("thr", [128, 5], F32)
        sbg_t = sbp("sbg_t", [128, 1], F32)
        dsg_t = sbp("dsg_t", [128, 2], F32)
        gsc = sbp("gsc", [128, 2], F32)
        lamb = sbp("lamb", [128, 4, 128], F32)
        lamw = sbp("lamw", [128, 2, 128], F32)
        lams = sbp("lams", [128, 4], F32)
        neglam = sbp("neglam", [128, 1], F32)
        es_c = ExitStack()
        tmpc = lambda name, shape, dtype: es_c.enter_context(nc.sbuf_tensor(name, shape, dtype))
        it_i = tmpc("it_i", [128, 512], I32)
        it_f = tmpc("it_f", [128, 512], F32)
        tv_i = tmpc("tv_i", [128, 512], I32)
        tv_f = tmpc("tv_f", [128, 512], F32)
        pid_i = tmpc("pid_i", [128, 1], I32)

        t0 = s.op('pool', lambda e: e.iota(it_i[:], pattern=[[1, 512]], base=0, channel_multiplier=-1))
        t1 = s.op('pool', lambda e: e.iota(tv_i[:], pattern=[[1, 512]], base=0, channel_multiplier=0))
        t2 = s.op('pool', lambda e: e.iota(pid_i[:], pattern=[[0, 1]], base=0, channel_multiplier=1))
        c0 = s.op('dve', lambda e: e.tensor_copy(out=it_f[:], in_=it_i[:]), [t0])
        c1 = s.op('dve', lambda e: e.tensor_copy(out=tv_f[:], in_=tv_i[:]), [t1])
        c2 = s.op('dve', lambda e: e.tensor_copy(out=pidx[:], in_=pid_i[:]), [t2])
        s.op('dve', lambda e: e.tensor_single_scalar(out=identf[:], in_=it_f[:, 0:128], scalar=0.0, op=ALU.is_equal), [c0])
        s.op('dve', lambda e: e.tensor_single_scalar(out=identb[:], in_=it_f[:, 0:128], scalar=0.0, op=ALU.is_equal), [c0])
        s.op('dve', lambda e: e.tensor_scalar(out=negU[:], in0=it_f[:, 0:128], scalar1=0.0, scalar2=-1.0,
                                              op0=ALU.is_le, op1=ALU.mult), [c0])
        s.op('dve', lambda e: e.memset(negOnes[:], -1.0))
        s.op('dve', lambda e: e.memset(onesf[:], 1.0))
        for j in range(4):
            s.op('dve', lambda e, j=j: e.tensor_single_scalar(out=msb[:, j, :], in_=it_f[:], scalar=128.0 * j,
                                                              op=ALU.is_gt), [c0])
        s.op('dve', lambda e: e.tensor_scalar(out=thr[:, 4:5], in0=pidx[:], scalar1=64.0, scalar2=64.0,
                                              op0=ALU.is_ge, op1=ALU.mult), [c2])
        for j in range(4):
            tj = s.op('dve', lambda e, j=j: e.tensor_scalar_add(out=thr[:, j:j + 1], in0=thr[:, 4:5], scalar1=128.0 * j),
                      [s.last('dve')])
            s.op('dve', lambda e, j=j: e.tensor_scalar(out=mdf[:, j, :], in0=tv_f[:], scalar1=thr[:, j:j + 1], scalar2=None,
                                                       op0=ALU.is_ge), [tj, c1])
        cst_tok = s.last('dve')
        es_c.close()
        s.fence()

        dsP = s.dsem()
        s.dma('sync', sbg_t[:], sbg, dsP)
        s.dma('sync', dsg_t[:], dsg, dsP)
        for i in range(4):
            s.dma('sync', lamb[:, i, :], lam4[i:i + 1, :].partition_broadcast(128), dsP)
        ptok = (dsP.sem, dsP.count)
        s.op('dve', lambda e: e.tensor_scalar_mul(out=gsc[:], in0=dsg_t[:], scalar1=1.0 - LAM_INIT), [ptok])
        s.op('dve', lambda e: e.tensor_tensor(out=lamw[:, 0, :], in0=lamb[:, 0, :], in1=lamb[:, 1, :], op=ALU.mult), [ptok])
        s.op('dve', lambda e: e.tensor_tensor(out=lamw[:, 1, :], in0=lamb[:, 2, :], in1=lamb[:, 3, :], op=ALU.mult), [ptok])
        l0 = s.op('dve', lambda e: e.reduce_sum(out=lams[:, 0:2], in_=lamw[:], axis=AX.X), [s.last('dve')])
        l1 = s.op('act', lambda e: e.activation(out=lams[:, 2:4], in_=lams[:, 0:2], func=AF.Exp), [l0])
        l2 = s.op('dve', lambda e: e.tensor_tensor(out=neglam[:], in0=lams[:, 3:4], in1=lams[:, 2:3], op=ALU.subtract), [l1])
        s.op('dve', lambda e: e.tensor_scalar_add(out=neglam[:], in0=neglam[:], scalar1=-LAM_INIT), [l2])

        es_cv = ExitStack()
        sb2c = lambda name, shape, dtype: es_cv.enter_context(nc.sbuf_tensor(name, shape, dtype))
        stg = [sb2c("cv_f%d" % i, [128, 4, 512], F32) for i in range(3)]
        obf = [sb2c("cv_b%d" % i, [128, 4, 512], BF16) for i in range(2)]
        dsl_c = [s.dsem() for _ in range(3)]
        dso_c = [s.dsem() for _ in range(3)]
        cast_eng = ['pool', 'act']
        jobs = []
        Wgu_flat = Wgu_loc.rearrange("a r c -> (a r) c")
        Wd_flat = Wd_loc.rearrange("a r c -> (a r) c")
        for i in range(8):
            for m, srct in enumerate([wgm, wum]):
                for q in range(4):
                    ch = i * 4 + q
                    srcv = srct[ch * 512:(ch + 1) * 512, :].rearrange("(r p) c -> p r c", p=128)
                    r0 = (i * 2 + m) * DM + q * 512
                    dstv = Wgu_flat[r0:r0 + 512, :].rearrange("(r p) c -> p r c", p=128)
                    jobs.append((srcv, dstv))
            for q in range(4):
                ch = i * 4 + q
                srcv = wdm[ch * 128:(ch + 1) * 128, :].rearrange("p (r c) -> p r c", c=512)
                dstv = Wd_flat[ch * 128:(ch + 1) * 128, :].rearrange("p (r c) -> p r c", c=512)
                jobs.append((srcv, dstv))
        cast_tok = [None] * 3
        out_tok = [None] * 2
        jobctr = [0]
        GRP4 = [[0, 1, 2, 3], [4, 5, 6, 7]]
        ld_tok = {}

        def conv_load(n):
            if n >= len(jobs):
                return
            k = n % 3
            ld_tok[n] = s.dma('pool', stg[k][:], jobs[n][0], dsl_c[k], [cast_tok[k]])

        def conv_step(nj):
            for _ in range(nj):
                n = jobctr[0]
                if n >= len(jobs):
                    return
                jobctr[0] += 1
                if n == 0:
                    conv_load(0)
                    conv_load(1)
                conv_load(n + 2)
                srcv, dstv = jobs[n]
                k = n % 3
                ko = n % 2
                ct = s.op('pool', lambda e, k=k, ko=ko: e.tensor_copy(out=obf[ko][:], in_=stg[k][:]), [ld_tok.pop(n), out_tok[ko]])
                cast_tok[k] = ct
                out_tok[ko] = s.dma('pool', dstv, obf[ko][:], dso_c[ko], [ct])
                if n % 12 == 11:
                    i = n // 12
                    s._wait('pool', [t for t in out_tok if t is not None])
                    for q in range(4):
                        nc.gpsimd.collective_compute("AllGather", ALU.bypass, replica_groups=GRP4,
                                                     ins=[Wgu_loc[i * 4 + q]], outs=[Wgu_all[i * 4 + q]]).then_inc(cc_sem)
                        cc_cnt[0] += 1
                    for q in range(2):
                        nc.gpsimd.collective_compute("AllGather", ALU.bypass, replica_groups=GRP4,
                                                     ins=[Wd_loc[i * 2 + q]], outs=[Wd_all[i * 2 + q]]).then_inc(cc_sem)
                        cc_cnt[0] += 1

        def conv_finish():
            conv_step(len(jobs))
            return (cc_sem, cc_cnt[0])

        with ExitStack() as es2:
            sb2 = lambda name, shape, dtype: es2.enter_context(nc.sbuf_tensor(name, shape, dtype))
            pm = sb2("pm", [128, 1], F32)
            invf = sb2("invf", [128, 1], F32)
            sgn = sb2("sgn", [128, 1], F32)
            posi = sb2("posi", [128, 2048], I32)
            posf = sb2("posf", [128, 2048], F32)
            ang = sb2("ang", [128, 2048], F32)
            r1 = sb2("r1", [128, 2048], F32)
            r2 = sb2("r2", [128, 2048], F32)
            sn = sb2("sn", [128, 2048], F32)
            cs = sb2("cs", [128, 2048], F32)
            dsr = s.dsem()
            s.op('dve', lambda e: e.tensor_scalar(out=pm[:], in0=pidx[:], scalar1=64.0, scalar2=-64.0, op0=ALU.is_ge, op1=ALU.mult), [c2])
            a0 = s.op('dve', lambda e: e.tensor_tensor(out=pm[:], in0=pm[:], in1=pidx[:], op=ALU.add), [s.last('dve')])
            a1 = s.op('act', lambda e: e.activation(out=invf[:], in_=pm[:], func=AF.Exp, scale=-math.log(10000.0) / 64.0), [a0])
            s.op('dve', lambda e: e.tensor_scalar(out=sgn[:], in0=pidx[:], scalar1=64.0, scalar2=2.0, op0=ALU.is_ge, op1=ALU.mult), [c2])
            a2 = s.op('dve', lambda e: e.tensor_scalar_add(out=sgn[:], in0=sgn[:], scalar1=-1.0), [s.last('dve')])
            wtok = None
            for ch in range(4):
                p0 = s.op('pool', lambda e, ch=ch: e.iota(posi[:], pattern=[[1, 2048]], base=ch * 2048, channel_multiplier=0),
                          [s.last('dve')])
                p1 = s.op('dve', lambda e: e.tensor_copy(out=posf[:], in_=posi[:]), [p0])
                p2 = s.op('dve', lambda e: e.tensor_scalar_mul(out=ang[:], in0=posf[:], scalar1=invf[:, 0:1]), [p1, a1, s.last('act')])
                def reduce_turns(dst, off):
                    s.op('dve', lambda e: e.tensor_scalar(out=dst[:], in0=ang[:], scalar1=1.0 / (2 * PI), scalar2=off, op0=ALU.mult, op1=ALU.add), [p2], seq=True)
                    s.op('dve', lambda e: e.tensor_copy(out=posi[:], in_=dst[:]), seq=True)
                    s.op('dve', lambda e: e.tensor_copy(out=posf[:], in_=posi[:]), seq=True)
                    s.op('dve', lambda e: e.tensor_tensor(out=dst[:], in0=dst[:], in1=posf[:], op=ALU.subtract), seq=True)
                    s.op('dve', lambda e: e.tensor_single_scalar(out=posf[:], in_=dst[:], scalar=0.5, op=ALU.is_gt), seq=True)
                    s.op('dve', lambda e: e.tensor_tensor(out=dst[:], in0=dst[:], in1=posf[:], op=ALU.subtract), seq=True)
                    s.op('dve', lambda e: e.tensor_single_scalar(out=posf[:], in_=dst[:], scalar=-0.5, op=ALU.is_lt), seq=True)
                    s.op('dve', lambda e: e.tensor_tensor(out=dst[:], in0=dst[:], in1=posf[:], op=ALU.add), seq=True)
                    return s.op('dve', lambda e: e.tensor_scalar_mul(out=dst[:], in0=dst[:], scalar1=2 * PI - 1e-5), seq=True)
                p3 = reduce_turns(r1, 0.0)
                p4 = reduce_turns(r2, 0.25)
                q1 = s.op('act', lambda e: e.activation(out=sn[:], in_=r1[:], func=AF.Sin), [p3, wtok])
                q2 = s.op('act', lambda e: e.activation(out=cs[:], in_=r2[:], func=AF.Sin), [p4, wtok])
                q3 = s.op('dve', lambda e: e.tensor_scalar_mul(out=sn[:], in0=sn[:], scalar1=sgn[:, 0:1]), [q1, a2])
                s.dma('sync', sinT[:, ch * 2048:(ch + 1) * 2048], sn[:], dsr, [q3])
                wtok = s.dma('sync', cosT[:, ch * 2048:(ch + 1) * 2048], cs[:], dsr, [q2])
            rot_tok = wtok
            s.fence([rot_tok])

        scr_tok = []
        with ExitStack() as es2:
            sb2 = lambda name, shape, dtype: es2.enter_context(nc.sbuf_tensor(name, shape, dtype))
            wsb = sb2("wsb", [128, 16, 2048], BF16)
            xst = [sb2("xst%d" % i, [128, DM], F32) for i in range(2)]
            hb = [sb2("hb%d" % i, [128, DM], BF16) for i in range(4)]
            hT = [sb2("hT%d" % i, [128, 16, 512], BF16) for i in range(2)]
            g1b = sb2("g1b", [128, DM], F32)
            ssq = sb2("ssq", [128, 4], F32)
            rstd = sb2("rstd", [128, 2], F32)
            cst = [sb2("cst0", [128, 512], F32)] * 2
            snt = [sb2("snt0", [128, 512], F32)] * 2
            qst = [sb2("qst%d" % i, [128, 512], BF16) for i in range(8)]
            vst = [sb2("vst%d" % i, [128, 512], BF16) for i in range(4)]
            rt1 = [sb2("rt1_%d" % i, [128, 512], F32) for i in range(2)]
            rt2 = [sb2("rt2_%d" % i, [128, 512], F32) for i in range(2)]
            dsx = [s.dsem() for _ in range(2)]
            dsw = [s.dsem() for _ in range(2)]
            dsg1 = s.dsem()
            dstab = [s.dsem()] * 2
            dsq = [s.dsem() for _ in range(8)]
            dsv = [s.dsem() for _ in range(4)]
            gtok = s.dma('sync', g1b[:], g1.partition_broadcast(128), dsg1)
            ctk = [None, None]
            for kt in range(16):
                k = kt % 2
                ld = s.dma('sync', xst[k][:], win[kt * 128:(kt + 1) * 128, :], dsw[k], [ctk[k]])
                ce = 'dve' if k == 0 else 'pool'
                ctk[k] = s.op(ce, lambda e, k=k, kt=kt: e.tensor_copy(out=wsb[:, kt, :], in_=xst[k][:]), [ld])
            wtoks = list(ctk)
            xfree = list(ctk)
            hbfree = [None] * 4
            hh_tok = {}
            hTfree = [None, None]
            tpfree = [None] * 4
            qfree = [None] * 8
            vfree = [None] * 4
            hTready = [None, None]

            def prep_front(gi):
                for j in range(4):
                    i = gi * 4 + j
                    k = i % 2
                    k4 = i % 4
                    ld = s.dma('sync', xst[k][:], xb[i * 128:(i + 1) * 128, :], dsx[k], [xfree[k]])
                    sq = s.op('act', lambda e, k=k, k4=k4: e.activation(out=hb[k4][:], in_=xst[k][:], func=AF.Square,
                                                                       accum_out=ssq[:, k:k + 1]), [ld, hbfree[k4]])
                    r = rstd_chain(s, rstd[:, k:k + 1], ssq[:, k:k + 1], 1.0 / DM, [sq, xfree[k]])
                    hh = s.op('dve', lambda e, k=k, k4=k4: e.scalar_tensor_tensor(out=hb[k4][:], in0=xst[k][:], scalar=rstd[:, k:k + 1],
                                                                             in1=g1b[:], op0=ALU.mult, op1=ALU.mult), [r, gtok])
                    xfree[k] = hh
                    hh_tok[i] = hh

            def prep_back(gi):
                hsel = gi % 2
                evs = []
                for j in range(4):
                    i = gi * 4 + j
                    k4 = i % 4
                    hh = hh_tok.pop(i)
                    tp_ = j % 2
                    tpv = ps_all[:, tp_ * 1024:(tp_ + 1) * 1024].bitcast(BF16).rearrange("p (k t) -> p k t", t=128)
                    for kt in range(16):
                        last = s.op('pe', lambda e, k4=k4, kt=kt, tpv=tpv: e.transpose(out=tpv[:, kt, :], in_=hb[k4][:, kt * 128:(kt + 1) * 128],
                                                                                      identity=identb[:]),
                                    [hh, tpfree[2 * tp_], tpfree[2 * tp_ + 1], cst_tok], signal=(kt == 15))
                    hbfree[k4] = last
                    e0 = s.op('act', lambda e, j=j, tpv=tpv: e.copy(out=hT[hsel][:, 0:8, j * 128:(j + 1) * 128], in_=tpv[:, 0:8, :]),
                              [last, hTfree[hsel]])
                    e1 = s.op('dve', lambda e, j=j, tpv=tpv: e.tensor_copy(out=hT[hsel][:, 8:16, j * 128:(j + 1) * 128], in_=tpv[:, 8:16, :]),
                              [last, hTfree[hsel]])
                    tpfree[2 * tp_], tpfree[2 * tp_ + 1] = e0, e1
                    evs += [e0, e1]
                hTready[hsel] = evs

            mmbank = [4, 5, 6, 7]
            bankfree = {b: None for b in mmbank}
            bctr = [0]

            def nextbank():
                b = mmbank[bctr[0] % len(mmbank)]
                bctr[0] += 1
                return b

            def mm(gi):
                hsel = gi % 2
                h = hT[hsel]
                rdy = hTready[hsel]
                t0c = gi * 512
                tb = gi % 2
                tabt = [s.dma('sync', cst[tb][:], cosT[:, t0c:t0c + 512], dstab[tb], [rot_tok, tabfree[tb]]),
                        s.dma('sync', snt[tb][:], sinT[:, t0c:t0c + 512], dstab[tb], [rot_tok, tabfree[tb]])]
                tabt = [(dstab[tb].sem, dstab[tb].count)]
                lastmm = None

                def blockmm(blk):
                    b = nextbank()
                    for kt in range(16):
                        t = s.op('pe', lambda e, kt=kt, b=b, blk=blk: e.matmul(bank(b), lhsT=wsb[:, kt, blk * 128:(blk + 1) * 128],
                                                                               rhs=h[:, kt, :], start=(kt == 0), stop=(kt == 15)),
                                 rdy + wtoks + [bankfree[b]], signal=(kt == 15))
                    return b, t
                for blk in range(4):
                    b, t = blockmm(blk)
                    if blk < 2:
                        ev = s.op('act', lambda e, b=b, blk=blk: e.activation(out=qst[blk][:], in_=bank(b), func=AF.Copy, scale=SCALE),
                                  [t, qfree[blk]])
                    else:
                        ev = s.op('act', lambda e, b=b, blk=blk: e.copy(out=qst[blk][:], in_=bank(b)), [t, qfree[blk]])
                    bankfree[b] = ev
                    qfree[blk] = s.dma('sync', QK[blk, :, t0c:t0c + 512], qst[blk][:], dsq[blk], [ev])
                    lastmm = t
                for blk in range(4, 8):
                    ba, ta = blockmm(blk)
                    bb, tb_ = blockmm(blk + 4)
                    r = blk % 2
                    u1 = s.op('dve', lambda e, ba=ba, r=r: e.tensor_tensor(out=rt1[r][:], in0=bank(ba), in1=cst[tb][:], op=ALU.mult),
                              [ta] + tabt + [rtfree[r]])
                    bankfree[ba] = u1
                    u2 = s.op('dve', lambda e, bb=bb, r=r: e.tensor_tensor(out=rt2[r][:], in0=bank(bb), in1=snt[tb][:], op=ALU.mult),
                              [tb_] + tabt)
                    bankfree[bb] = u2
                    u3 = s.op('pool', lambda e, r=r, blk=blk: e.tensor_tensor(out=qst[blk][:], in0=rt1[r][:], in1=rt2[r][:], op=ALU.add),
                              [u1, u2, qfree[blk]])
                    rtfree[r] = u3
                    qfree[blk] = s.dma('sync', QK[blk, :, t0c:t0c + 512], qst[blk][:], dsq[blk], [u3])
                    lastmm = tb_
                tabfree[tb] = s.last('dve')
                for j in range(4):
                    b = nextbank()
                    for kt in range(16):
                        t = s.op('pe', lambda e, kt=kt, b=b, j=j: e.matmul(bank(b), lhsT=h[:, kt, j * 128:(j + 1) * 128],
                                                                           rhs=wsb[:, kt, 1536:2048], start=(kt == 0), stop=(kt == 15)),
                                 rdy + wtoks + [bankfree[b]], signal=(kt == 15))
                    ev = s.op('act', lambda e, b=b, j=j: e.copy(out=vst[j][:], in_=bank(b)), [t, vfree[j]])
                    bankfree[b] = ev
                    r0 = t0c + j * 128
                    vfree[j] = s.dma('sync', VV[r0:r0 + 128, :], vst[j][:], dsv[j], [ev])
                    lastmm = t
                hTfree[hsel] = lastmm

            tabfree = _One([None])
            rtfree = [None, None]
            NG = 16
            prep_front(0)
            prep_back(0)
            for gi in range(NG):
                if gi + 1 < NG:
                    prep_front(gi + 1)
                mm(gi)
                if gi + 1 < NG:
                    prep_back(gi + 1)
            scr_tok = [t for t in qfree + vfree if t is not None]
            s.fence(scr_tok)

        if STAGE <= 1:
            es_cv.close()
            return nc, finish(nc, s, es, y, scr_tok)

        mix_tok = []
        dsm = [s.dsem() for _ in range(4)]
        mixdone = lambda: [(d.sem, d.count) for d in dsm if d.count > 0]
        with ExitStack() as es2:
            sb2 = lambda name, shape, dtype: es2.enter_context(nc.sbuf_tensor(name, shape, dtype))
            qT = [sb2("qT%d" % i, [128, SEQ], BF16) for i in range(2)]
            kT = [sb2("kT%d" % i, [128, SEQ], BF16) for i in range(2)]
            vS = [sb2("vS%d" % i, [128, 64, 128], BF16) for i in range(2)]
            e_sb = [sb2("e_sb%d" % i, [128, 512], F32) for i in range(2)]
            Lp = [sb2("Lp%d" % i, [128, 512], BF16) for i in range(3)]
            Ls = [sb2("Ls%d" % i, [128, 512], BF16) for i in range(3)]
            A_sb = [sb2("A_sb%d" % i, [128, 512], BF16) for i in range(2)]
            o_sb = sb2("o_sb", [128, 512], F32)
            sq_sb = sb2("sq_sb", [128, 512], F32)
            rs_sb = sb2("rs_sb", [128, 512], F32)
            mo = [sb2("mo%d" % i, [128, 512], BF16) for i in range(2)]
            dsl = s.dsem()
            for hh in range(2):
                s.dma('sync', qT[hh][:], QK[hh], dsl)
                s.dma('sync', kT[hh][:], QK[2 + hh], dsl)
                s.dma('sync', vS[hh][:], VV[:, hh * 128:(hh + 1) * 128].rearrange("(n p) d -> p n d", p=128), dsl)
            ldtok = (dsl.sem, dsl.count)
            zfree = [None, None, None]
            ofree = [None, None]
            finfree = [None]
            efree = [None, None]
            lpfree = [None, None, None]
            lsfree = [None, None, None]
            asbfree = [None, None]
            mofree = [None, None]
            osbfree = [None]
            blocks = []
            for hh in range(2):
                for qt in range(16):
                    kbs = list(range(4 * qt + 3, -1, -1))
                    for n, kb in enumerate(kbs):
                        blocks.append(dict(h=hh, qt=qt, kb=kb, first=(n == 0), last=(n == len(kbs) - 1), diag=(kb - 4 * qt)))
            if STAGE == 2:
                blocks = [b for b in blocks if b['h'] == 0 and b['qt'] < 2]
            st = {}
            oidx = [0]
            lsw = [0]

            def sb_z(i):
                B = blocks[i]
                z = i % 3
                h, qt, kb = B['h'], B['qt'], B['kb']
                st.setdefault(i, {})
                st[i]['z'] = s.op('pe', lambda e: e.matmul(bank(z), lhsT=kT[h][:, kb * 128:(kb + 1) * 128],
                                                           rhs=qT[h][:, qt * 512:(qt + 1) * 512], start=True, stop=False),
                                  [ldtok, zfree[z]])

            def sb_act1(i):
                B = blocks[i]
                z = i % 3
                ez = i % 2
                l3 = i % 3
                t = s.op('act', lambda e: e.activation(out=e_sb[ez][:], in_=bank(z), func=AF.Exp), [st[i]['z'], efree[ez]])
                t2 = s.op('act', lambda e: e.activation(out=Lp[l3][:], in_=e_sb[ez][:], func=AF.Ln, bias=1.0), [t, lpfree[l3]])
                efree[ez] = t2
                if B['diag'] >= 0:
                    j = B['diag']
                    t2 = s.op('dve', lambda e: e.tensor_tensor(out=Lp[l3][:], in0=Lp[l3][:], in1=msb[:, j, :], op=ALU.mult), [t2, cst_tok])
                st[i]['lp'] = t2
                if not B['last']:
                    if B['first']:
                        nxt = (lsw[0] + 1) % 3
                        t3 = s.op('dve', lambda e: e.tensor_copy(out=Ls[nxt][:], in_=Lp[l3][:]), [t2, lsfree[nxt]])
                    else:
                        cur = st[i]['ls_in']
                        nxt = (cur + 1) % 3
                        t3 = s.op('dve', lambda e: e.tensor_tensor(out=Ls[nxt][:], in0=Ls[cur][:], in1=Lp[l3][:], op=ALU.add),
                                  [t2, lsfree[nxt], st[i]['ls_tok']])
                    lsw[0] = nxt
                    st.setdefault(i + 1, {})
                    st[i + 1]['ls_in'] = nxt
                    st[i + 1]['ls_tok'] = t3

            def sb_arg(i):
                B = blocks[i]
                z = i % 3
                l3 = i % 3
                lastflag = B['first']
                t = s.op('pe', lambda e: e.matmul(bank(z), lhsT=negU[:], rhs=Lp[l3][:], start=False, stop=lastflag),
                         [st[i]['lp'], cst_tok])
                if not B['first']:
                    cur = st[i]['ls_in']
                    t = s.op('pe', lambda e: e.matmul(bank(z), lhsT=negOnes[:], rhs=Ls[cur][:], start=False, stop=True),
                             [st[i]['ls_tok']])
                    lsfree[cur] = t
                lpfree[l3] = t
                st[i]['arg'] = t

            def sb_act2(i):
                B = blocks[i]
                z = i % 3
                a = i % 2
                t = s.op('act', lambda e: e.activation(out=A_sb[a][:], in_=bank(z), func=AF.Exp), [st[i]['arg'], asbfree[a]])
                zfree[z] = t
                if B['diag'] >= 0:
                    j = B['diag']
                    t = s.op('dve', lambda e: e.tensor_tensor(out=A_sb[a][:], in0=A_sb[a][:], in1=msb[:, j, :], op=ALU.mult), [t])
                st[i]['A'] = t

            def sb_av(i):
                B = blocks[i]
                a = i % 2
                h, qt, kb = B['h'], B['qt'], B['kb']
                if B['first']:
                    oidx[0] += 1
                ob = 4 + (oidx[0] % 2)
                t = s.op('pe', lambda e: e.matmul(bank(ob), lhsT=vS[h][:, kb, :], rhs=A_sb[a][:], start=B['first'], stop=B['last']),
                         [st[i]['A'], ofree[oidx[0] % 2] if B['first'] else None])
                asbfree[a] = t
                if B['last']:
                    sb_fin(h, qt, ob, oidx[0] % 2, t)
                del st[i]

            def sb_fin(h, qt, ob, oi, tok):
                c1 = s.op('act', lambda e: e.copy(out=o_sb[:], in_=bank(ob)), [tok, osbfree[0]])
                ofree[oi] = c1
                c2 = s.op('act', lambda e: e.activation(out=sq_sb[:], in_=o_sb[:], func=AF.Square), [c1, finfree[0]])
                m = s.op('pe', lambda e: e.matmul(bank(6), lhsT=onesf[:], rhs=sq_sb[:], start=True, stop=True), [c2, finfree[0], cst_tok])
                r = rstd_chain(s, rs_sb[:], bank(6), 1.0 / 128.0, [m, osbfree[0]])
                finfree[0] = r
                mi = (h * 16 + qt) % 2
                f = s.op('dve', lambda e: e.scalar_tensor_tensor(out=mo[mi][:], in0=o_sb[:], scalar=sbg_t[:, 0:1], in1=rs_sb[:],
                                                                 op0=ALU.mult, op1=ALU.mult), [r, c1, ptok, mofree[mi]])
                osbfree[0] = f
                mofree[mi] = s.dma('sync', mix_loc[qt, h * 128:(h + 1) * 128, :], mo[mi][:], dsm[mi], [f])
                mix_tok.append(mofree[mi])

            nb = len(blocks)
            for step in range(nb + 2):
                if step < nb:
                    sb_z(step)
                    sb_act1(step)
                if 1 <= step <= nb:
                    sb_arg(step - 1)
                    sb_act2(step - 1)
                if step >= 2:
                    sb_av(step - 2)
                if step % 10 == 5 and STAGE >= 4:
                    conv_step(1)
            wts_tok = conv_finish() if STAGE >= 4 else None
            s.fence(mixdone() + [t for t in out_tok if t is not None])

        es_cv.close()
        if STAGE <= 2:
            return nc, finish(nc, s, es, y, mixdone())

        with ExitStack() as es2:
            sb2 = lambda name, shape, dtype: es2.enter_context(nc.sbuf_tensor(name, shape, dtype))
            qD = [sb2("qD%d" % i, [128, SEQ], BF16) for i in range(2)]
            kD = [sb2("kD%d" % i, [128, SEQ], BF16) for i in range(2)]
            vD = sb2("vD", [128, 64, 256], BF16)
            E_sb = [sb2("E_sb%d" % i, [128, 512], BF16) for i in range(4)]
            Es = [[sb2("Es%d_%d" % (i, pp), [128, 512], F32) for pp in range(2)] for i in range(2)]
            rc = [sb2("rc%d" % i, [128, 512], F32) for i in range(2)]
            tA = sb2("tA", [128, 512], F32)
            tB = sb2("tB", [128, 512], F32)
            dh = [sb2("dh%d" % i, [128, 512], F32) for i in range(2)]
            sqd = sb2("sqd", [128, 512], F32)
            rsd = sb2("rsd", [128, 512], F32)
            mod_ = [sb2("mod%d" % i, [128, 512], BF16) for i in range(2)]
            dsl = s.dsem()
            for c in range(2):
                s.dma('sync', qD[c][:], QK[4 + c], dsl)
                s.dma('sync', kD[c][:], QK[6 + c], dsl)
            s.dma('sync', vD[:], VV[:, 256:512].rearrange("(n p) d -> p n d", p=128), dsl)
            ldtok = (dsl.sem, dsl.count)
            sfree = [None, None, None]
            Efree = [None] * 4
            Efree2 = [None] * 4
            obfree = [None] * 4
            esfree = [[None, None], [None, None]]
            finfree = [None]
            modfree = [None, None]
            dblocks = []
            for qt in range(16):
                kbs = list(range(0, 4 * qt + 4))
                for n, kb in enumerate(kbs):
                    dblocks.append(dict(qt=qt, kb=kb, first=(n == 0), last=(n == len(kbs) - 1), diag=(kb - 4 * qt)))
            if STAGE == 3:
                dblocks = [b for b in dblocks if b['qt'] < 2]
            std = {}
            sctr = [0]
            ectr = [0]

            def d_s(i):
                B = dblocks[i]
                std[i] = {}
                for c in range(2):
                    sbk = sctr[0] % 3
                    sctr[0] += 1
                    t = s.op('pe', lambda e, c=c, sbk=sbk: e.matmul(bank(sbk), lhsT=kD[c][:, B['kb'] * 128:(B['kb'] + 1) * 128],
                                                                   rhs=qD[c][:, B['qt'] * 512:(B['qt'] + 1) * 512], start=True, stop=True),
                             [ldtok, sfree[sbk]])
                    std[i][('s', c)] = (sbk, t)

            def d_act(i):
                B = dblocks[i]
                for c in range(2):
                    sbk, t = std[i][('s', c)]
                    ei = ectr[0] % 4
                    ectr[0] += 1
                    t2 = s.op('act', lambda e, sbk=sbk, ei=ei: e.activation(out=E_sb[ei][:], in_=bank(sbk), func=AF.Exp, scale=SCALE),
                              [t, Efree[ei], Efree2[ei]])
                    sfree[sbk] = t2
                    if B['diag'] >= 0:
                        j = B['diag']
                        t2 = s.op('dve', lambda e, ei=ei, j=j: e.tensor_tensor(out=E_sb[ei][:], in0=E_sb[ei][:], in1=mdf[:, j, :], op=ALU.mult),
                                  [t2, cst_tok])
                    aeng = 'dve' if c == 0 else 'pool'
                    pq = B['qt'] % 2
                    if B['first']:
                        t3 = s.op(aeng, lambda e, ei=ei, c=c, pq=pq: e.tensor_copy(out=Es[c][pq][:], in_=E_sb[ei][:]), [t2, esfree[c][pq]])
                    else:
                        t3 = s.op(aeng, lambda e, ei=ei, c=c, pq=pq: e.tensor_tensor(out=Es[c][pq][:], in0=Es[c][pq][:], in1=E_sb[ei][:], op=ALU.add),
                                  [t2, s.last(aeng)])
                    Efree2[ei] = t3
                    std[i][('E', c)] = (ei, t2, t3)

            def d_av(i):
                B = dblocks[i]
                lasts = []
                for c in range(2):
                    ei, t2, t3 = std[i][('E', c)]
                    for hf in range(2):
                        ob = 3 + 2 * c + hf
                        t = s.op('pe', lambda e, ob=ob, hf=hf, ei=ei: e.matmul(bank(ob), lhsT=vD[:, B['kb'], hf * 128:(hf + 1) * 128],
                                                                              rhs=E_sb[ei][:], start=B['first'], stop=B['last']),
                                 [t2, obfree[2 * c + hf] if B['first'] else None])
                    Efree[ei] = t
                    std[i][('p', c)] = t3
                    lasts.append((t, t3))
                if B['last']:
                    d_fin(B['qt'], lasts)
                del std[i]

            def d_fin(qt, lasts):
                for c in range(2):
                    pe_t, pool_t = lasts[c]
                    m = s.op('pe', lambda e, c=c: e.matmul(bank(7), lhsT=onesf[:], rhs=Es[c][qt % 2][:], start=True, stop=True),
                             [pool_t, finfree[0], cst_tok])
                    esfree[c][qt % 2] = m
                    r = s.op('dve', lambda e, c=c: e.reciprocal(out=rc[c][:], in_=bank(7)), [m])
                    finfree[0] = r
                lastpe = lasts[1][0]
                for hf in range(2):
                    a = s.op('dve', lambda e, hf=hf: e.tensor_tensor(out=tA[:], in0=bank(3 + hf), in1=rc[0][:], op=ALU.mult), [lastpe, s.last('dve')])
                    obfree[hf] = a
                    b_ = s.op('dve', lambda e, hf=hf: e.tensor_tensor(out=tB[:], in0=bank(5 + hf), in1=rc[1][:], op=ALU.mult), [lastpe, a])
                    obfree[2 + hf] = b_
                    s.op('dve', lambda e, hf=hf: e.scalar_tensor_tensor(out=dh[hf][:], in0=tB[:], scalar=neglam[:, 0:1], in1=tA[:],
                                                                        op0=ALU.mult, op1=ALU.add), [b_, s.last('pe')])
                dtok = s.last('dve')
                for hf in range(2):
                    c2 = s.op('act', lambda e, hf=hf: e.activation(out=sqd[:], in_=dh[hf][:], func=AF.Square), [dtok, s.last('pe')])
                    m = s.op('pe', lambda e, hf=hf: e.matmul(bank(7), lhsT=onesf[:], rhs=sqd[:], start=(hf == 0), stop=(hf == 1)),
                             [c2, finfree[0]])
                r = rstd_chain(s, rsd[:], bank(7), 1.0 / 256.0, [m, modfree[0], modfree[1]])
                finfree[0] = r
                for hf in range(2):
                    f = s.op('dve', lambda e, hf=hf: e.scalar_tensor_tensor(out=mod_[hf][:], in0=dh[hf][:], scalar=gsc[:, hf:hf + 1], in1=rsd[:],
                                                                            op0=ALU.mult, op1=ALU.mult), [r, modfree[hf]])
                    modfree[hf] = s.dma('sync', mix_loc[qt, 256 + hf * 128:256 + (hf + 1) * 128, :], mod_[hf][:], dsm[2 + hf], [f])
                    dq_tok.append(modfree[hf])
                if STAGE >= 4:
                    s._wait('pool', dq_tok[-2:] + mixdone())
                    nc.gpsimd.collective_compute("AllGather", ALU.bypass, replica_groups=[[0, 1, 2, 3], [4, 5, 6, 7]],
                                                 ins=[mix_loc[qt]], outs=[mix_all[qt]]).then_inc(cc_sem)
                    cc_cnt[0] += 1

            nb = len(dblocks)
            dq_tok = []
            for step in range(nb + 1):
                if step < nb:
                    d_s(step)
                    d_act(step)
                if step >= 1:
                    d_av(step - 1)
            s.fence(mixdone())

        if STAGE <= 3:
            return nc, finish(nc, s, es, y, mixdone())

        mixall_tok = (cc_sem, cc_cnt[0])
        if STAGE == 35:
            return nc, finish(nc, s, es, y, mixdone() + [mixall_tok])

        comb = sbp("comb", [128, 16, 32], F32)
        c1_toks = []
        with ExitStack() as es2:
            sb2 = lambda name, shape, dtype: es2.enter_context(nc.sbuf_tensor(name, shape, dtype))
            mixT = sb2("mixT", [128, 16, NTOK], BF16)
            wo = sb2("wo", [128, 16, DM], BF16)
            g2b = sb2("g2b", [128, DM], F32)
            wr_sb = sb2("wr_sb", [128, 16, 36], F32)
            brb = sb2("brb", [128, 36], F32)
            selb = sb2("selb", [128, 4], F32)
            xs_t = [sb2("xs_t0", [128, DM], F32)] * 2
            x1 = [sb2("x1_0", [128, DM], F32)] * 2
            h2 = [sb2("h2_0", [128, DM], F32)] * 2
            wst = h2
            jk = sb2("jk", [128, DM], BF16)
            h2lo = sb2("h2lo", [128, DM], BF16)
            h2Tl = sb2("h2Tl", [128, 16, 128], BF16)
            wr_hi = sb2("wr_hi", [128, 16, 36], BF16)
            wr_lo = sb2("wr_lo", [128, 16, 36], BF16)
            h2Tb = [sb2("h2Tb%d" % i, [128, 16, 128], BF16) for i in range(2)]
            ssq2 = sb2("ssq2", [128, 2], F32)
            rstd2 = sb2("rstd2", [128, 2], F32)
            lg = sb2("lg", [128, 36], F32)
            rt = sb2("rt", [128, 64], F32)
            dsA = s.dsem()
            dsW = [s.dsem()] * 2
            dsG = [s.dsem() for _ in range(2)]
            dsX = [s.dsem()] * 2
            dsX1 = [s.dsem()] * 2
            dsH = [s.dsem() for _ in range(2)]
            s.dma('sync', g2b[:], g2.partition_broadcast(128), dsA)
            s.dma('sync', wr_sb[:], wr.rearrange("(k p) c -> p k c", p=128), dsA)
            s.dma('sync', brb[:], br.partition_broadcast(128), dsA)
            s.dma('sync', selb[:], sel.partition_broadcast(128), dsA)
            atok = (dsA.sem, dsA.count)
            w1 = s.op('dve', lambda e: e.tensor_copy(out=wr_hi[:], in_=wr_sb[:]), [atok])
            wrtok = s.op('dve', lambda e: e.tensor_tensor(out=wr_lo[:], in0=wr_sb[:], in1=wr_hi[:], op=ALU.subtract), [w1])
            jkfree = [None]
            ctk = [None, None]
            for kt in range(16):
                k = kt % 2
                ld = s.dma('sync', wst[k][:], wout[kt * 128:(kt + 1) * 128, :], dsW[k], [ctk[0], ctk[1]])
                ce = 'dve' if k == 0 else 'pool'
                ctk[k] = s.op(ce, lambda e, k=k, kt=kt: e.tensor_copy(out=wo[:, kt, :], in_=wst[k][:]), [ld])
            wotoks = list(ctk)
            gt = None
            selbuf = [jk, h2lo]
            bufree = [None, None]
            nsel = 0
            for kt in range(16):
                for j in range(4):
                    sbuf_ = selbuf[nsel % 2]
                    ld = s.dma('sync', sbuf_[:].rearrange("p (q t) -> p q t", q=4),
                               mix_all[4 * j:4 * j + 4, kt * 128:(kt + 1) * 128, :].rearrange("q p t -> p q t"), dsG[nsel % 2],
                               [mixall_tok, bufree[nsel % 2]])
                    if j == 0:
                        gt = s.op('dve', lambda e, kt=kt, sbuf_=sbuf_: e.tensor_scalar_mul(out=mixT[:, kt, :], in0=sbuf_[:], scalar1=selb[:, 0:1]), [ld, atok])
                    else:
                        gt = s.op('dve', lambda e, kt=kt, j=j, sbuf_=sbuf_: e.scalar_tensor_tensor(out=mixT[:, kt, :], in0=sbuf_[:], scalar=selb[:, j:j + 1],
                                                                                              in1=mixT[:, kt, :], op0=ALU.mult, op1=ALU.add), [ld, gt])
                    bufree[nsel % 2] = gt
                    nsel += 1
            gtok = gt
            xfree = _One([None])
            x1free = _One([None])
            h2free = _One([None])
            hbfree = [None, None]
            psfree = [None]
            tpfree = [None]
            tpfree2 = [None]
            h32free = [None]
            for tt in range(C1T):
                k = tt % 2
                ld = s.dma('sync', xs_t[k][:], xs[tt * 128:(tt + 1) * 128, :], dsX[k], [xfree[k]])
                for c in range(4):
                    for kt in range(16):
                        t = s.op('pe', lambda e, c=c, kt=kt: e.matmul(bank(c), lhsT=mixT[:, kt, tt * 128:(tt + 1) * 128],
                                                                      rhs=wo[:, kt, c * 512:(c + 1) * 512], start=(kt == 0), stop=(kt == 15)),
                                 [gtok] + wotoks + [psfree[0]], signal=(c == 3 and kt == 15))
                a = s.op('dve', lambda e, k=k: e.tensor_tensor(out=x1[k][:], in0=ps_all[:, 0:2048], in1=xs_t[k][:], op=ALU.add),
                         [t, ld, x1free[k]])
                psfree[0] = a
                xfree[k] = a
                st1 = s.dma('sync', X1[tt * 128:(tt + 1) * 128, :], x1[k][:], dsX1[k], [a])
                sq = s.op('act', lambda e, k=k: e.activation(out=jk[:], in_=x1[k][:], func=AF.Square, accum_out=ssq2[:, k:k + 1]), [a, jkfree[0]])
                r = rstd_chain(s, rstd2[:, k:k + 1], ssq2[:, k:k + 1], 1.0 / DM, [sq, h2free[k]])
                hh = s.op('dve', lambda e, k=k: e.scalar_tensor_tensor(out=h2[k][:], in0=x1[k][:], scalar=rstd2[:, k:k + 1], in1=g2b[:],
                                                                       op0=ALU.mult, op1=ALU.mult), [r, atok, h2free[k]])
                x1free[k] = st1
                xfree[k] = hh
                if C1R == 0:
                    c1_toks.append(st1)
                    continue
                hi_t = s.op('act', lambda e, k=k: e.copy(out=jk[:], in_=h2[k][:]), [hh, jkfree[0]])
                lo_t = s.op('dve', lambda e, k=k: e.tensor_tensor(out=h2lo[:], in0=h2[k][:], in1=jk[:], op=ALU.subtract), [hi_t, jkfree[0]])
                h2free[k] = lo_t
                tpv = ps_all[:, 2048:4096].bitcast(BF16).rearrange("p (k t) -> p k t", t=128)
                for kt in range(16):
                    s.op('pe', lambda e, kt=kt: e.transpose(out=tpv[:, kt, :], in_=jk[:, kt * 128:(kt + 1) * 128], identity=identb[:]),
                         [hi_t, tpfree[0], tpfree2[0], cst_tok], signal=False)
                for kt in range(16):
                    tp = s.op('pe', lambda e, kt=kt: e.transpose(out=tpv[:, 16 + kt, :], in_=h2lo[:, kt * 128:(kt + 1) * 128], identity=identb[:]),
                              [lo_t], signal=(kt == 15))
                jkfree[0] = tp
                e1 = s.op('dve', lambda e, k=k: e.tensor_copy(out=h2Tb[k][:], in_=tpv[:, 0:16, :]), [tp, hbfree[k], h32free[0]])
                e0 = s.op('act', lambda e: e.copy(out=h2Tl[:], in_=tpv[:, 16:32, :]), [tp, h32free[0]])
                tpfree[0] = e1
                tpfree2[0] = e0
                if os.environ.get('MK_NOH2T'):
                    hbfree[k] = e1
                else:
                    hbfree[k] = s.dma('sync', H2T.rearrange("(k p) t -> p k t", p=128)[:, :, tt * 128:(tt + 1) * 128], h2Tb[k][:], dsH[k], [e1])
                c1_toks.append(hbfree[k])
                c1_toks.append(st1)
                if C1R == 1:
                    continue
                combos = [(h2Tb[k], wr_hi), (h2Tl, wr_hi), (h2Tb[k], wr_lo)]
                for ci, (lh, rw) in enumerate(combos):
                    for kt in range(16):
                        lt = s.op('pe', lambda e, kt=kt, lh=lh, rw=rw, ci=ci: e.matmul(ps_all[:, 0:36], lhsT=lh[:, kt, :], rhs=rw[:, kt, :],
                                                                                   start=(ci == 0 and kt == 0), stop=(ci == 2 and kt == 15)),
                                  [e0, e1, a, wrtok], signal=(ci == 2 and kt == 15))
                h32free[0] = lt
                lgt = s.op('dve', lambda e: e.tensor_tensor(out=lg[:], in0=ps_all[:, 0:36], in1=brb[:], op=ALU.add), [lt])
                psfree[0] = lgt
                V = nc.vector
                cmb = comb[:, tt, :]
                s.op('dve', lambda e: e.reduce_max(out=rt[:, 0:1], in_=lg[:, 0:4], axis=AX.X), [lgt], seq=True)
                s.op('dve', lambda e: e.tensor_scalar(out=rt[:, 8:12], in0=lg[:, 0:4], scalar1=rt[:, 0:1], scalar2=None, op0=ALU.is_equal), seq=True)
                s.op('dve', lambda e: e.tensor_scalar(out=rt[:, 1:5], in0=lg[:, 0:4], scalar1=rt[:, 0:1], scalar2=None, op0=ALU.subtract), seq=True)
                d1 = s.op('dve', lambda e: e.tensor_scalar_mul(out=rt[:, 16:24], in0=lg[:, 4:12], scalar1=rt[:, 8:9]), seq=True)
                ex = s.op('act', lambda e: e.activation(out=rt[:, 1:5], in_=rt[:, 1:5], func=AF.Exp), [d1])
                for gq in range(1, 4):
                    s.op('dve', lambda e, gq=gq: e.scalar_tensor_tensor(out=rt[:, 16:24], in0=lg[:, 4 + 8 * gq:12 + 8 * gq], scalar=rt[:, 8 + gq:9 + gq],
                                                                        in1=rt[:, 16:24], op0=ALU.mult, op1=ALU.add), seq=True)
                s.op('dve', lambda e: e.reduce_max(out=rt[:, 24:25], in_=rt[:, 16:24], axis=AX.X), seq=True)
                d2 = s.op('dve', lambda e: e.tensor_scalar(out=rt[:, 32:40], in0=rt[:, 16:24], scalar1=rt[:, 24:25], scalar2=None, op0=ALU.subtract), seq=True)
                ex2 = s.op('act', lambda e: e.activation(out=rt[:, 32:40], in_=rt[:, 32:40], func=AF.Exp), [d2])
                s.op('dve', lambda e: e.reduce_sum(out=rt[:, 5:6], in_=rt[:, 1:5], axis=AX.X), [ex, ex2], seq=True)
                s.op('dve', lambda e: e.reciprocal(out=rt[:, 6:7], in_=rt[:, 5:6]), seq=True)
                s.op('dve', lambda e: e.reduce_max(out=rt[:, 40:41], in_=rt[:, 32:40], axis=AX.X), seq=True)
                s.op('dve', lambda e: e.tensor_scalar(out=rt[:, 41:49], in0=rt[:, 32:40], scalar1=rt[:, 40:41], scalar2=None, op0=ALU.is_equal), seq=True)
                s.op('dve', lambda e: e.scalar_tensor_tensor(out=rt[:, 49:57], in0=rt[:, 41:49], scalar=-4.0, in1=rt[:, 32:40],
                                                             op0=ALU.mult, op1=ALU.add), seq=True)
                s.op('dve', lambda e: e.reduce_max(out=rt[:, 57:58], in_=rt[:, 49:57], axis=AX.X), seq=True)
                s.op('dve', lambda e: e.tensor_tensor(out=rt[:, 58:59], in0=rt[:, 40:41], in1=rt[:, 57:58], op=ALU.add), seq=True)
                s.op('dve', lambda e: e.reciprocal(out=rt[:, 58:59], in_=rt[:, 58:59]), seq=True)
                s.op('dve', lambda e: e.tensor_tensor(out=rt[:, 58:59], in0=rt[:, 58:59], in1=rt[:, 6:7], op=ALU.mult), seq=True)
                s.op('dve', lambda e: e.tensor_tensor(out=rt[:, 59:60], in0=rt[:, 40:41], in1=rt[:, 58:59], op=ALU.mult), seq=True)
                s.op('dve', lambda e: e.tensor_tensor(out=rt[:, 60:61], in0=rt[:, 57:58], in1=rt[:, 58:59], op=ALU.mult), seq=True)
                s.op('dve', lambda e: e.tensor_scalar(out=rt[:, 49:57], in0=rt[:, 49:57], scalar1=rt[:, 57:58], scalar2=rt[:, 60:61],
                                                      op0=ALU.is_equal, op1=ALU.mult), seq=True)
                s.op('dve', lambda e: e.scalar_tensor_tensor(out=rt[:, 49:57], in0=rt[:, 41:49], scalar=rt[:, 59:60], in1=rt[:, 49:57],
                                                             op0=ALU.mult, op1=ALU.add), seq=True)
                for gq in range(4):
                    s.op('dve', lambda e, gq=gq: e.tensor_scalar_mul(out=comb[:, tt, gq * 8:(gq + 1) * 8], in0=rt[:, 49:57],
                                                                     scalar1=rt[:, 8 + gq:9 + gq]), seq=True)
            comb_tok = s.last('dve')
            s.fence(c1_toks + [comb_tok])

        if STAGE <= 4:
            return nc, finish(nc, s, es, y, c1_toks)

        with ExitStack() as es2:
            sb2 = lambda name, shape, dtype: es2.enter_context(nc.sbuf_tensor(name, shape, dtype))
            g3b = sb2("g3b", [128, DM], F32)
            dsg3 = s.dsem()
            g3tok = s.dma('sync', g3b[:], g3.partition_broadcast(128), dsg3)
            hg = [sb2("hg0", [128, 16, 512], BF16)] * 2
            wgu = [sb2("wgu%d" % i, [128, 2, 16, 512], BF16) for i in range(2)]
            wdd = [sb2("wdd%d" % i, [128, 4, DM], BF16) for i in range(2)]
            acc = [sb2("acc%d" % i, [128, DM], F32) for i in range(4)]
            sg = [sb2("sg%d" % i, [128, 512], F32) for i in range(2)]
            hTe = [sb2("hTe%d" % i, [128, 4, 512], BF16) for i in range(2)]
            x1t = [sb2("x1t0", [128, DM], F32)] * 2
            yo = [sb2("yo0", [128, DM], F32)] * 2
            jk2 = sb2("jk2", [128, DM], BF16)
            ssq3 = sb2("ssq3", [128, 2], F32)
            rstd3 = sb2("rstd3", [128, 2], F32)
            dsHg = [s.dsem()] * 2
            dsWt = [s.dsem() for _ in range(2)]
            dsXl = [s.dsem()] * 2
            dsY = [s.dsem()] * 2
            NGRP = 4
            NEXP = NE if STAGE >= 6 else 2
            wfree = [None, None]
            hgfree = _One([None])
            hTefree = [None, None]
            sgfree = [None, None]
            accfree = [None] * 4
            x1tfree = _One([None])
            yofree = _One([None])
            gubank = [0, 1, 2, 3]
            gufree = [None] * 4
            dnfree = [None] * 4
            H2Tv = H2T.rearrange("(k p) t -> p k t", p=128)
            ectr = 0
            yt = []
            for grp in range(NGRP):
                hs = grp % 2
                hl = s.dma('sync', hg[hs][:], H2Tv[:, :, grp * 512:(grp + 1) * 512], dsHg[hs], [hgfree[hs]] + c1_toks[-4:])
                lastpe_grp = None
                for ex in range(NEXP):
                    wsel = ectr % 2
                    ectr += 1
                    rk, il = ex // 8, ex % 8
                    for m in range(2):
                        for hq in range(2):
                            chk = il * 4 + m * 2 + hq
                            s.dma('sync', wgu[wsel][:, m, hq * 8:(hq + 1) * 8, :],
                                  Wgu_all[chk, rk * 1024:(rk + 1) * 1024, :].rearrange("(k p) c -> p k c", p=128), dsWt[wsel],
                                  [wfree[wsel], wts_tok])
                    for hq in range(2):
                        chk = il * 2 + hq
                        s.dma('sync', wdd[wsel][:, hq * 2:(hq + 1) * 2, :],
                              Wd_all[chk, rk * 256:(rk + 1) * 256, :].rearrange("(k p) c -> p k c", p=128), dsWt[wsel],
                              [wfree[wsel], wts_tok])
                    wl = (dsWt[wsel].sem, dsWt[wsel].count)
                    hsel = ex % 2
                    for jt in range(4):
                        bg = (2 * jt) % 4
                        bu = (2 * jt + 1) % 4
                        for m, b in ((0, bg), (1, bu)):
                            for kt in range(16):
                                t = s.op('pe', lambda e, m=m, b=b, kt=kt, jt=jt: e.matmul(bank(b), lhsT=wgu[wsel][:, m, kt, jt * 128:(jt + 1) * 128],
                                                                                         rhs=hg[hs][:, kt, :], start=(kt == 0), stop=(kt == 15)),
                                         [wl, hl, gufree[b]], signal=(kt == 15))
                            if m == 0:
                                tg = t
                            else:
                                tu = t
                        si = jt % 2
                        a1 = s.op('act', lambda e, bg=bg, si=si: e.activation(out=sg[si][:], in_=bank(bg), func=AF.Silu), [tg, sgfree[si]])
                        gufree[bg] = a1
                        a2 = s.op('dve', lambda e, bu=bu, si=si, jt=jt: e.tensor_tensor(out=hTe[hsel][:, jt, :], in0=bank(bu), in1=sg[si][:], op=ALU.mult),
                                  [tu, a1, hTefree[hsel]])
                        gufree[bu] = a2
                        sgfree[si] = a2
                    hready = a2
                    for tl in range(4):
                        tt = grp * 4 + tl
                        for c in range(4):
                            b = 4 + c
                            for jt in range(4):
                                t = s.op('pe', lambda e, b=b, jt=jt, tl=tl, c=c: e.matmul(bank(b), lhsT=hTe[hsel][:, jt, tl * 128:(tl + 1) * 128],
                                                                                         rhs=wdd[wsel][:, jt, c * 512:(c + 1) * 512], start=(jt == 0), stop=(jt == 3)),
                                         [hready, dnfree[c]], signal=(jt == 3))
                            eidx = ex
                            if ex == 0:
                                u = s.op('dve', lambda e, b=b, tl=tl, c=c, tt=tt, eidx=eidx: e.tensor_scalar_mul(
                                    out=acc[tl][:, c * 512:(c + 1) * 512], in0=bank(b), scalar1=comb[:, tt, eidx:eidx + 1]), [t, accfree[tl], comb_tok])
                            else:
                                u = s.op('dve', lambda e, b=b, tl=tl, c=c, tt=tt, eidx=eidx: e.scalar_tensor_tensor(
                                    out=acc[tl][:, c * 512:(c + 1) * 512], in0=bank(b), scalar=comb[:, tt, eidx:eidx + 1],
                                    in1=acc[tl][:, c * 512:(c + 1) * 512], op0=ALU.mult, op1=ALU.add), [t])
                            dnfree[c] = u
                        lastpe_grp = t
                    wfree[wsel] = lastpe_grp
                    hTefree[hsel] = lastpe_grp
                hgfree[hs] = lastpe_grp
                for tl in range(4):
                    tt = grp * 4 + tl
                    k = tt % 2
                    ld = s.dma('sync', x1t[k][:], X1[tt * 128:(tt + 1) * 128, :], dsXl[k], [x1tfree[k]] + c1_toks)
                    a = s.op('dve', lambda e, k=k, tl=tl: e.tensor_tensor(out=x1t[k][:], in0=x1t[k][:], in1=acc[tl][:], op=ALU.add), [ld, s.last('dve')])
                    accfree[tl] = a
                    sq = s.op('act', lambda e, k=k: e.activation(out=jk2[:], in_=x1t[k][:], func=AF.Square, accum_out=ssq3[:, k:k + 1]), [a])
                    r = rstd_chain(s, rstd3[:, k:k + 1], ssq3[:, k:k + 1], 1.0 / DM, [sq, yofree[k]])
                    f = s.op('dve', lambda e, k=k: e.scalar_tensor_tensor(out=yo[k][:], in0=x1t[k][:], scalar=rstd3[:, k:k + 1], in1=g3b[:],
                                                                          op0=ALU.mult, op1=ALU.mult), [r, g3tok, yofree[k]])
                    x1tfree[k] = f
                    yofree[k] = s.dma('sync', y[tt * 128:(tt + 1) * 128, :], yo[k][:], dsY[k], [f])
                    yt.append(yofree[k])
            return nc, finish(nc, s, es, y, yt)


def finish(nc, s, es, y, toks):
    print('SCHED counts', s.cnt, 'ndsem', s.nds, flush=True)
    s.fence(toks)
    return None


_CACHE = {}


def kernel(**inputs):
    x = np.ascontiguousarray(inputs["x"], dtype=np.float32)
    w_in = np.asarray(inputs["w_in"], dtype=np.float32)[0]
    w_out = np.asarray(inputs["w_out"], dtype=np.float32)[0]
    wg = np.asarray(inputs["w_gate"], dtype=np.float32)[0]
    wu = np.asarray(inputs["w_up"], dtype=np.float32)[0]
    wd = np.asarray(inputs["w_down"], dtype=np.float32)[0]
    f = lambda k: np.asarray(inputs[k], dtype=np.float32)
    if "nc" not in _CACHE:
        _CACHE["nc"] = build_program()[0]
    nc = _CACHE["nc"]
    wr = np.ascontiguousarray(np.concatenate([f("w_group_router")[0], f("w_expert_router")[0]], axis=1))
    br = np.ascontiguousarray(np.concatenate([f("b_group_router")[0], f("b_expert_router")[0]], axis=0)[None, :])
    lam4 = np.ascontiguousarray(np.stack([f("diff_lambda_q1")[0], f("diff_lambda_k1")[0], f("diff_lambda_q2")[0], f("diff_lambda_k2")[0]], 0))
    rows = []
    for i in range(4):
        rows += [np.arange(2 * i * 128, 2 * i * 128 + 256), np.arange(1024 + i * 256, 1024 + i * 256 + 256)]
    wout_p = np.ascontiguousarray(w_out[np.concatenate(rows), :])
    sw = np.concatenate([np.arange(64, 128), np.arange(0, 64)])
    in_maps = []
    for c in range(8):
        b, g = c // 4, c % 4
        cols = []
        for h in (2 * g, 2 * g + 1):
            cols.append(np.arange(h * 128, h * 128 + 128))
        for h in (2 * g, 2 * g + 1):
            cols.append(1024 + np.arange(h * 128, h * 128 + 128))
        dq0 = 3072 + g * 256
        dk0 = 4096 + g * 256
        for base in (dq0, dq0 + 128, dk0, dk0 + 128):
            cols.append(base + np.arange(128))
        for base in (dq0, dq0 + 128, dk0, dk0 + 128):
            cols.append(base + sw)
        for h in (2 * g, 2 * g + 1):
            cols.append(2048 + np.arange(h * 128, h * 128 + 128))
        cols.append(5120 + g * 256 + np.arange(256))
        cols = np.concatenate(cols)
        assert cols.size == 2048
        dsg = f("diff_subln_gain")[0].reshape(2, 128).T
        gix = ((np.arange(16)[None, :] * 128 + np.arange(128)[:, None]) * 4 + g).astype(np.int32)
        in_maps.append({
            "xb": x[b],
            "xs": np.ascontiguousarray(x[b, g * NTOK:(g + 1) * NTOK]),
            "win": np.ascontiguousarray(w_in[:, cols]),
            "g1": f("attn_norm_gain"), "g2": f("ffn_norm_gain"), "g3": f("final_norm_gain")[None, :],
            "sbg": np.ascontiguousarray(f("sb_norm_gain")[0][:, None]),
            "dsg": np.ascontiguousarray(dsg),
            "lam4": lam4, "wout": wout_p, "wr": wr, "br": br,
            "wgm": np.ascontiguousarray(wg[8 * g:8 * g + 8].reshape(8 * DM, DE)),
            "wum": np.ascontiguousarray(wu[8 * g:8 * g + 8].reshape(8 * DM, DE)),
            "wdm": np.ascontiguousarray(wd[8 * g:8 * g + 8].reshape(8 * DE, DM)),
            "sel": np.eye(4, dtype=np.float32)[g:g + 1].copy(),
        })
    res = run_bass_kernel_spmd(nc, in_maps, core_ids=list(range(8)))
    _CACHE["res"] = res
    out = np.empty((2, SEQ, DM), np.float32)
    for c in range(8):
        b, g = c // 4, c % 4
        out[b, g * NTOK:(g + 1) * NTOK] = res.results[c]["y"]
    return out
```
